# Optimizing a Trainium2 kernel written in Bass

```python
import math
import jax, jax.numpy as jnp
from jax import lax
import numpy as np

D_MODEL = 1024
BATCH = 4
SEQ = 8192
DEPTH = 1

HEAD_DIM = 64
N_HEADS_DIL = 8
N_HEADS_FOX = 8
D_DIL = N_HEADS_DIL * HEAD_DIM
D_FOX = N_HEADS_FOX * HEAD_DIM
D_MIX = D_DIL + D_FOX
D_IN = 3 * D_DIL + 3 * D_FOX + N_HEADS_FOX
DILATED_BRANCHES = ((128, 1), (512, 4), (2048, 16))
SEG = 128
BLOCK_Q = 128
N_BUCKETS = 32
MAX_DISTANCE = 2048
N_GROUPS = 4
EXPERTS_PER_GROUP = 8
N_EXPERTS = N_GROUPS * EXPERTS_PER_GROUP
TOP_K_IN_GROUP = 2
D_EXPERT = 256
EPS = 1e-6
SCALE = HEAD_DIM ** -0.5
NEG_INF = -1e30

kernel_name = "hymba_dilated_fox_hmoe_layer"


def _rmsnorm(x, g):
    x32 = x.astype(jnp.float32)
    y = x32 * lax.rsqrt(jnp.mean(x32 * x32, axis=-1, keepdims=True) + EPS)
    return (y * g.astype(jnp.float32)).astype(x.dtype)


def _t5_bucket(dist):
    max_exact = N_BUCKETS // 2
    d32 = jnp.maximum(dist, 1).astype(jnp.float32)
    large = max_exact + (jnp.log(d32 / max_exact) / math.log(MAX_DISTANCE / max_exact)
                         * (N_BUCKETS - max_exact)).astype(jnp.int32)
    large = jnp.minimum(large, N_BUCKETS - 1)
    return jnp.where(dist < max_exact, dist, large)


def _dilated_branch(q, k, v, rel_bias, dilation):
    B, T, H, Dh = q.shape
    L = T // dilation
    nb = -(-L // SEG)
    L_pad = nb * SEG

    def to_sub(a):
        a = a.reshape(B, L, dilation, H, Dh).transpose(0, 2, 1, 3, 4)
        a = jnp.pad(a, ((0, 0), (0, 0), (0, L_pad - L), (0, 0), (0, 0)))
        return a.reshape(B, dilation, nb, SEG, H, Dh)

    qs, ks, vs = to_sub(q), to_sub(k), to_sub(v)

    def with_prev(a):
        prev = jnp.pad(a, ((0, 0), (0, 0), (1, 0), (0, 0), (0, 0), (0, 0)))[:, :, :-1]
        return jnp.concatenate([prev, a], axis=3)

    kk, vv = with_prev(ks), with_prev(vs)

    i = jnp.arange(SEG)[:, None]
    j = jnp.arange(2 * SEG)[None, :]
    m = i - j + SEG
    band = (m >= 0) & (m <= SEG)
    bias = rel_bias[_t5_bucket(jnp.clip(m, 0, SEG) * dilation)]
    bias = bias.transpose(2, 0, 1).astype(jnp.float32)
    blk = jnp.arange(nb)[:, None, None]
    valid = band[None] & (blk * SEG - SEG + j[None] >= 0)

    s = jnp.einsum('brnqhd,brnkhd->brnhqk', qs, kk,
                   preferred_element_type=jnp.float32) * SCALE + bias
    s = jnp.where(valid[:, None], s, NEG_INF)
    mx = jnp.max(s, axis=-1, keepdims=True)
    p = jnp.exp(s - mx)
    den = jnp.sum(p, axis=-1, keepdims=True)
    o = jnp.einsum('brnhqk,brnkhd->brnqhd', (p / den).astype(v.dtype), vv)
    lse = (mx + jnp.log(den))[..., 0].transpose(0, 1, 2, 4, 3)

    def from_sub(a):
        a = a.reshape(B, dilation, L_pad, *a.shape[4:])[:, :, :L]
        a = jnp.moveaxis(a, 1, 2)
        return a.reshape(B, T, *a.shape[3:])

    return from_sub(o), from_sub(lse)


def _dilated_attention(q, k, v, rel_bias):
    outs, lses = [], []
    for _, dilation in DILATED_BRANCHES:
        o, l = _dilated_branch(q, k, v, rel_bias, dilation)
        outs.append(o)
        lses.append(l)
    w = jax.nn.softmax(jnp.stack(lses), axis=0)
    return jnp.einsum('gbth,gbthd->bthd', w.astype(q.dtype), jnp.stack(outs))


def _forgetting_attention(q, k, v, log_f):
    B, T, H, Dh = q.shape
    nq = T // BLOCK_Q
    c = jnp.cumsum(log_f, axis=1)
    c_k = c.transpose(0, 2, 1)
    k_pos = jnp.arange(T)
    qb = q.reshape(B, nq, BLOCK_Q, H, Dh).transpose(1, 0, 2, 3, 4)
    cb = c.reshape(B, nq, BLOCK_Q, H).transpose(1, 0, 3, 2)
    starts = jnp.arange(nq) * BLOCK_Q

    def block(args):
        qi, ci, t0 = args
        s = jnp.einsum('bqhd,bkhd->bhqk', qi, k, preferred_element_type=jnp.float32) * SCALE
        s = s + ci[..., None] - c_k[:, :, None, :]
        q_pos = t0 + jnp.arange(BLOCK_Q)
        s = jnp.where(k_pos[None, :] <= q_pos[:, None], s, NEG_INF)
        p = jax.nn.softmax(s, axis=-1)
        return jnp.einsum('bhqk,bkhd->bqhd', p.astype(v.dtype), v)

    o = lax.map(block, (qb, cb, starts))
    return o.transpose(1, 0, 2, 3, 4).reshape(B, T, H, Dh)


def _hierarchical_moe(xt, w_rg, b_rg, w_re, b_re, w_gate, w_up, w_down):
    N = xt.shape[0]
    group_logits = (xt @ w_rg + b_rg).astype(jnp.float32)
    group_probs = jax.nn.softmax(group_logits, axis=-1)
    g1, g_idx = lax.top_k(group_probs, 1)
    expert_logits = (xt @ w_re + b_re).astype(jnp.float32).reshape(N, N_GROUPS, EXPERTS_PER_GROUP)
    sel = jnp.take_along_axis(expert_logits, g_idx[:, :, None], axis=1)[:, 0]
    top_v, top_i = lax.top_k(sel, TOP_K_IN_GROUP)
    g2 = jax.nn.softmax(top_v, axis=-1)
    expert_id = g_idx * EXPERTS_PER_GROUP + top_i
    combine = jnp.sum(jax.nn.one_hot(expert_id, N_EXPERTS, dtype=jnp.float32)
                      * (g1 * g2)[..., None], axis=1).astype(xt.dtype)
    y = jnp.zeros_like(xt)
    for e in range(N_EXPERTS):
        he = jax.nn.silu(xt @ w_gate[e]) * (xt @ w_up[e])
        y = y + combine[:, e:e + 1] * (he @ w_down[e])
    return y


def setup_inputs(seed: int = 0) -> dict:
    key = jax.random.key(seed)
    ks = jax.random.split(key, 20)
    f32 = jnp.float32
    nrm = lambda k, shape, s: jax.random.normal(k, shape, f32) * s
    return {
        "x": nrm(ks[0], (BATCH, SEQ, D_MODEL), 1.0),
        "w_in": nrm(ks[1], (DEPTH, D_MODEL, D_IN), D_MODEL ** -0.5),
        "b_forget": 2.0 + nrm(ks[2], (DEPTH, N_HEADS_FOX), 0.5),
        "attn_norm": 1.0 + nrm(ks[3], (DEPTH, D_MODEL), 0.02),
        "out_norm_dil": 1.0 + nrm(ks[4], (DEPTH, D_DIL), 0.02),
        "out_norm_fox": 1.0 + nrm(ks[5], (DEPTH, D_FOX), 0.02),
        "w_out": nrm(ks[6], (DEPTH, D_MIX, D_MODEL), D_MIX ** -0.5),
        "ffn_norm": 1.0 + nrm(ks[7], (DEPTH, D_MODEL), 0.02),
        "w_router_group": nrm(ks[8], (DEPTH, D_MODEL, N_GROUPS), D_MODEL ** -0.5),
        "b_router_group": nrm(ks[9], (DEPTH, N_GROUPS), 0.01),
        "w_router_expert": nrm(ks[10], (DEPTH, D_MODEL, N_EXPERTS), D_MODEL ** -0.5),
        "b_router_expert": nrm(ks[11], (DEPTH, N_EXPERTS), 0.01),
        "w_expert_gate": nrm(ks[12], (DEPTH, N_EXPERTS, D_MODEL, D_EXPERT), D_MODEL ** -0.5),
        "w_expert_up": nrm(ks[13], (DEPTH, N_EXPERTS, D_MODEL, D_EXPERT), D_MODEL ** -0.5),
        "w_expert_down": nrm(ks[14], (DEPTH, N_EXPERTS, D_EXPERT, D_MODEL), D_EXPERT ** -0.5),
        "rel_bias": nrm(ks[15], (N_BUCKETS, N_HEADS_DIL), 0.5),
        "final_norm": 1.0 + nrm(ks[16], (D_MODEL,), 0.02),
    }


def reference(x, w_in, b_forget, attn_norm, out_norm_dil, out_norm_fox, w_out, ffn_norm,
              w_router_group, b_router_group, w_router_expert, b_router_expert,
              w_expert_gate, w_expert_up, w_expert_down, rel_bias, final_norm):
    B, T, D = x.shape
    splits = [D_DIL, 2 * D_DIL, 3 * D_DIL, 3 * D_DIL + D_FOX, 3 * D_DIL + 2 * D_FOX,
              3 * D_DIL + 3 * D_FOX]
    for layer in range(DEPTH):
        h = _rmsnorm(x, attn_norm[layer])
        proj = h @ w_in[layer]
        qa, ka, va, qf, kf, vf, f_pre = jnp.split(proj, splits, axis=-1)
        heads = lambda a, n: a.reshape(B, T, n, HEAD_DIM)
        o_dil = _dilated_attention(heads(qa, N_HEADS_DIL), heads(ka, N_HEADS_DIL),
                                   heads(va, N_HEADS_DIL), rel_bias)
        log_f = jax.nn.log_sigmoid((f_pre + b_forget[layer]).astype(jnp.float32))
        o_fox = _forgetting_attention(heads(qf, N_HEADS_FOX), heads(kf, N_HEADS_FOX),
                                      heads(vf, N_HEADS_FOX), log_f)
        o_dil = _rmsnorm(o_dil.reshape(B, T, D_DIL), out_norm_dil[layer])
        o_fox = _rmsnorm(o_fox.reshape(B, T, D_FOX), out_norm_fox[layer])
        x = x + jnp.concatenate([o_dil, o_fox], axis=-1) @ w_out[layer]
        h2 = _rmsnorm(x, ffn_norm[layer]).reshape(B * T, D)
        y = _hierarchical_moe(h2, w_router_group[layer], b_router_group[layer],
                              w_router_expert[layer], b_router_expert[layer],
                              w_expert_gate[layer], w_expert_up[layer], w_expert_down[layer])
        x = x + y.reshape(B, T, D)
    return _rmsnorm(x, final_norm)
```

```python
import math
import numpy as np
import ml_dtypes
import concourse.bass as bass
import concourse.mybir as mybir
from concourse.bass_utils import run_bass_kernel_spmd

F32 = mybir.dt.float32
BF16 = mybir.dt.bfloat16
I32 = mybir.dt.int32
AF = mybir.ActivationFunctionType
ALU = mybir.AluOpType
AX = mybir.AxisListType

ENGS = ("pe", "act", "dve", "pool", "sp")
EPS = 1e-6
NEGM = -30000.0
D = 1024
KC = 8
DIN = 3080
NEXP = 32
DEXP = 256
import os as _os
PIPE_B = _os.environ.get('PIPE_B', '1') == '1'
PIPE_C = _os.environ.get('PIPE_C', '1') == '1'
SKIP_D3 = _os.environ.get('SKIP_D3', '0') == '1'
NO_WALL = _os.environ.get('NO_WALL', '0') == '1'
SKIP_D1B = _os.environ.get('SKIP_D1B', '0') == '1'
SKIP_D2 = _os.environ.get('SKIP_D2', '0') == '1'
D2_STOP = int(_os.environ.get('D2_STOP', '0'))


class Prog:
    def __init__(self, nc, n_dma_slots=12):
        self.nc = nc
        self.ops = []
        self.eng_ops = {e: [] for e in ENGS}
        self.last_writer = {}
        self.readers = {}
        self.n_dma_slots = n_dma_slots
        self.dma_rr = {e: 0 for e in ENGS}
        self.slot_last = {}
        self.finals = []
        self.last_op = {}

    def _add(self, domain, eng, fn, reads, writes, is_dma):
        idx = len(self.ops)
        deps = set()
        raw = set()
        for k in reads:
            w = self.last_writer.get(k)
            if w is not None:
                deps.add(w)
                raw.add(w)
        for k in writes:
            w = self.last_writer.get(k)
            if w is not None:
                deps.add(w)
            for r in self.readers.get(k, {}).values():
                deps.add(r)
        for k in writes:
            self.last_writer[k] = idx
            self.readers[k] = {}
        for k in reads:
            self.readers.setdefault(k, {})[domain] = idx
        if is_dma:
            prev = self.slot_last.get(domain)
            if prev is not None:
                deps.add(prev)
            self.slot_last[domain] = idx
        deps.discard(idx)
        raw.discard(idx)
        self.ops.append([domain, eng, fn, sorted(deps), is_dma, raw])
        self.eng_ops[eng].append(idx)
        self.last_op[domain] = idx
        return idx

    def op(self, eng, fn, reads=(), writes=()):
        return self._add(eng, eng, fn, reads, writes, False)

    def dma(self, eng, out, in_, reads=(), writes=(), **kw):
        slot = self.dma_rr[eng]
        self.dma_rr[eng] = (slot + 1) % self.n_dma_slots
        domain = ("dma", eng, slot)
        fn = lambda e, out=out, in_=in_, kw=kw: e.dma_start(out=out, in_=in_, **kw)
        return self._add(domain, eng, fn, reads, writes, True)

    def dma_custom(self, eng, fn, reads=(), writes=()):
        slot = self.dma_rr[eng]
        self.dma_rr[eng] = (slot + 1) % self.n_dma_slots
        domain = ("dma", eng, slot)
        return self._add(domain, eng, fn, reads, writes, True)

    def barrier(self):
        lasts = dict(self.last_op)
        for eng in ENGS:
            idx = len(self.ops)
            deps = sorted(v for d, v in lasts.items() if d != eng)
            self.ops.append([eng, eng, None, deps, False, set()])
            self.eng_ops[eng].append(idx)
            self.last_op[eng] = idx

    def finalize(self):
        nc = self.nc
        ops = self.ops
        need = [False] * len(ops)
        waits = [None] * len(ops)
        for d in self.finals:
            need[d] = True
        for idx, o in enumerate(ops):
            if o[4]:
                need[idx] = True
        seen = {e: {} for e in ENGS}
        for idx, (domain, eng, fn, deps, is_dma, raw) in enumerate(ops):
            wl = []
            best = {}
            for d in deps:
                dd = ops[d][0]
                if dd == eng and not is_dma and not ops[d][4]:
                    if eng == "pe" or d not in raw:
                        continue
                if dd not in best or best[dd] < d:
                    best[dd] = d
            for dd, d in best.items():
                if seen[eng].get(dd, -1) >= d:
                    continue
                seen[eng][dd] = d
                wl.append(d)
                need[d] = True
            waits[idx] = wl
        self.sems = {}
        cnt = {}
        token = [None] * len(ops)
        for idx, (domain, eng, fn, deps, is_dma, raw) in enumerate(ops):
            if domain not in self.sems:
                name = "s_" + ("_".join(str(x) for x in domain) if isinstance(domain, tuple) else domain)
                self.sems[domain] = nc.alloc_semaphore(name)
                cnt[domain] = 0
            if need[idx]:
                cnt[domain] += 16 if is_dma else 1
                token[idx] = (self.sems[domain], cnt[domain])
        self.max_sem = max(cnt.values()) if cnt else 0
        self.token = token
        self.waits = waits
        return self

    def emit_engine(self, eng, e):
        ops = self.ops
        for idx in self.eng_ops[eng]:
            domain, _, fn, deps, is_dma, raw = ops[idx]
            for d in self.waits[idx]:
                sem, val = self.token[d]
                e.wait_ge(sem, val)
            if fn is None:
                if self.token[idx] is None:
                    continue
                ins = e.nop()
            else:
                ins = fn(e)
            if self.token[idx] is not None:
                sem, val = self.token[idx]
                ins.then_inc(sem, 16 if is_dma else 1)

    def run_block(self):
        nc = self.nc
        with nc.Block() as block:
            @block.sync
            def _(e):
                self.emit_engine("sp", e)
                for d in self.finals:
                    sem, val = self.token[d]
                    e.wait_ge(sem, val)

            @block.tensor
            def _(e):
                self.emit_engine("pe", e)

            @block.scalar
            def _(e):
                self.emit_engine("act", e)

            @block.vector
            def _(e):
                self.emit_engine("dve", e)

            @block.gpsimd
            def _(e):
                self.emit_engine("pool", e)


class SBAlloc:
    def __init__(self, nc):
        self.nc = nc
        self.base = (nc.sbuf_base + 63) // 64 * 64
        self.top = nc.sbuf_top
        self.off = self.base
        self.n = 0
        self.peak = 0

    def alloc(self, name, shape, dt):
        esz = 4 if dt in (F32, I32) else 2
        nbytes = int(np.prod(shape[1:])) * esz
        nbytes = (nbytes + 63) // 64 * 64
        self.n += 1
        t = self.nc.alloc_sbuf_tensor_at(f"{name}_{self.n}", list(shape), dt, offset=self.off)
        self.off += nbytes
        self.peak = max(self.peak, self.off)
        assert self.off <= self.top, f"SBUF overflow allocating {name}: {self.off} > {self.top}"
        return t

    def mark(self):
        return self.off

    def reset(self, m):
        self.off = m


def _t5_bucket_np(dist):
    max_exact = 16
    d32 = np.maximum(dist, 1).astype(np.float32)
    large = max_exact + (np.log(d32 / np.float32(max_exact)) / np.float32(math.log(2048 / max_exact))
                         * np.float32(32 - max_exact)).astype(np.int32)
    large = np.minimum(large, 31)
    return np.where(dist < max_exact, dist, large)


def _bias_tables(rel_bias):
    k = np.arange(128)[:, None]
    c = np.arange(256)[None, :]
    iq = np.where(c < 128, c, c - 128)
    m = np.where(c < 128, iq - k, iq - k + 128)
    valid = (m >= 0) & (m <= 128)
    mc = np.clip(m, 0, 128)
    btab = np.zeros((128, 24, 256), np.float32)
    for di, dil in enumerate((1, 4, 16)):
        bidx = _t5_bucket_np(mc * dil)
        for h in range(8):
            btab[:, di * 8 + h, :] = rel_bias[bidx, h]
    bmsk = valid.astype(np.float32)
    bneg = np.where(valid, 0.0, NEGM).astype(np.float32)
    return btab.reshape(128, 24 * 256), bmsk, bneg


def build_program(NPREV, NOWN, debug=False):
    assert NPREV % 2048 == 0 and NPREV >= 2048 and NOWN % 2048 == 0
    NTOK = NPREV + NOWN
    NT = NTOK // 128
    NTP = NPREV // 128
    NTO = NOWN // 128
    NG = NTOK // 512
    NCH = NOWN // 2048
    NQC = NOWN // 1024
    KD0 = NPREV - 2048
    NKD = NOWN + 2048

    nc = bass.Bass("TRN2", target_bir_lowering=False)
    NTL_ = 2 * NOWN // 128 + 32

    def din(name, shape, dt=F32):
        return nc.dram_tensor(name, list(shape), dt, kind="ExternalInput").ap()

    xs = din("xs", [NTOK, D])
    pvt = din("pvt", [128, 64])
    w_in = din("w_in", [D, DIN])
    b_forget = din("b_forget", [8])
    attn_norm = din("attn_norm", [D])
    out_norm_dil = din("out_norm_dil", [512])
    out_norm_fox = din("out_norm_fox", [512])
    w_out = din("w_out", [D, D])
    ffn_norm = din("ffn_norm", [D])
    w_rg = din("w_rg", [D, 4])
    b_rg = din("b_rg", [4])
    w_re = din("w_re", [D, 32])
    b_re = din("b_re", [32])
    w_eg = din("w_eg", [NEXP, D, DEXP])
    w_eu = din("w_eu", [NEXP, D, DEXP])
    w_ed = din("w_ed", [NEXP, DEXP, D])
    final_norm = din("final_norm", [D])
    btab_d = din("btab", [128, 24 * 256])
    bmsk_d = din("bmsk", [128, 256])
    bneg_d = din("bneg", [128, 256])
    identb_d = din("identb", [128, 128], BF16)
    identf_d = din("identf", [128, 128])
    tri_d = din("tri", [128, 128])
    mneg_d = din("mneg", [128, 128], BF16)
    NTL_ = 2 * NOWN // 128 + 32
    tris_d = din("tris", [128, 128])
    tokid_d = din("tokid", [128, NTO])
    jv_d = din("jv", [128, NTL_])
    pidx_d = din("pidx", [128, 1])
    slot_init_d = din("slot_init", [NTL_ * 128, 4], I32)
    y_out = nc.dram_tensor("y", [NOWN, D], F32, kind="ExternalOutput").ap()

    def dscr(name, shape, dt):
        return nc.dram_tensor(name, list(shape), dt).ap()

    QdT = dscr("QdT", [512, NOWN], BF16)
    KdT = dscr("KdT", [512, NKD], BF16)
    VdS = dscr("VdS", [NKD, 512], BF16)
    QfT = dscr("QfT", [512, NOWN], BF16)
    QfA = dscr("QfA", [8, 3, NOWN], BF16)
    KfT = dscr("KfT", [512, NTOK], BF16)
    VfS = dscr("VfS", [NTOK, 512], BF16)
    cS = dscr("cS", [NTOK, 8], F32)
    OcS = dscr("OcS", [1024, NOWN], BF16)
    WALL = dscr("WALL", [NEXP * 128, 6144], BF16)
    H2S = dscr("H2S", [NOWN, D], BF16)
    X1S = dscr("X1S", [NOWN, D], F32)
    YS = dscr("YS", [2 * NOWN + NTL_ * 128, D], F32)
    SLOTINFO = dscr("SLOTINFO", [NTL_ * 128, 4], I32)

    dbg = {}
    if debug:
        dbg["d_ctok"] = nc.dram_tensor("d_ctok", [128, NT * 8], F32, kind="ExternalOutput").ap()
        dbg["d_oc"] = nc.dram_tensor("d_oc", [1024, NOWN], F32, kind="ExternalOutput").ap()
        dbg["d_x1"] = nc.dram_tensor("d_x1", [NOWN, D], F32, kind="ExternalOutput").ap()
        dbg["d_comb"] = nc.dram_tensor("d_comb", [NOWN, 32], F32, kind="ExternalOutput").ap()
        dbg["d_slot"] = nc.dram_tensor("d_slot", [128, NTO * 2], F32, kind="ExternalOutput").ap()
        dbg["d_eid"] = nc.dram_tensor("d_eid", [128, NTL_], F32, kind="ExternalOutput").ap()
        dbg["d_cnt"] = nc.dram_tensor("d_cnt", [128, 32 * 3], F32, kind="ExternalOutput").ap()

    sb = SBAlloc(nc)
    ps = nc.alloc_psum_tensor("ps", [128, 8, 512], F32)
    p = Prog(nc)
    finals = []

    def psb(b, n=1):
        if n == 1:
            return ps[:, b, :]
        return ps[:, b:b + n, :].rearrange("p b c -> p (b c)")

    identb = sb.alloc("identb", [128, 128], BF16)
    identf = sb.alloc("identf", [128, 128], F32)
    mneg = sb.alloc("mneg", [128, 128], BF16)
    pvb = sb.alloc("pvb", [128, 64], BF16)
    pvf = sb.alloc("pvf", [128, 64], F32)
    ones_bf = sb.alloc("ones_bf", [128, 1], BF16)
    ctok = sb.alloc("ctok", [128, NT, 8], F32)
    crefbc = sb.alloc("crefbc", [128, NQC, 8], F32)
    p.dma("sp", identb[:], identb_d[:, :], writes=["identb"])
    p.dma("sp", identf[:], identf_d[:, :], writes=["identf"])
    p.dma("sp", mneg[:], mneg_d[:, :], writes=["mneg"])
    p.dma("sp", pvf[:], pvt[:, :], writes=["pvf"])
    p.op("dve", lambda e: e.tensor_copy(pvb[:], pvf[:]), reads=["pvf"], writes=["pvb"])
    p.op("dve", lambda e: e.memset(ones_bf[:], 1.0), writes=["ones_bf"])
    persist_mark = sb.mark()

    def phase_A():
        win = sb.alloc("win", [128, KC, DIN], BF16)
        gbc = sb.alloc("gbc", [128, D], F32)
        bfb = sb.alloc("bfb", [128, 8], F32)
        NXB, NHB = 4, 4
        xt = [sb.alloc(f"xt{i}", [128, D], F32) for i in range(NXB)]
        junk = sb.alloc("junk", [128, D], BF16)
        hb = [sb.alloc(f"hb{i}", [128, D], BF16) for i in range(NHB)]
        hT = [sb.alloc(f"hT{i}", [128, KC, 512], BF16) for i in range(2)]
        qst = [sb.alloc(f"qst{i}", [128, 512], BF16) for i in range(4)]
        vst = [sb.alloc(f"vst{i}", [128, 512], BF16) for i in range(4)]
        ssA = sb.alloc("ssA", [128, NT], F32)
        rsA = sb.alloc("rsA", [128, NT], F32)
        fall = sb.alloc("fall", [128, NT, 8], F32)
        tri = sb.alloc("tri", [128, 128], F32)
        onesf = sb.alloc("onesf", [128, 128], F32)

        for c in range(KC):
            p.dma("pool", win[:, c, :], w_in[c * 128:(c + 1) * 128, :], writes=[("win", c)])
        p.dma("sp", gbc[:], attn_norm.partition_broadcast(128), writes=["gbc"])
        p.dma("sp", bfb[:], b_forget.partition_broadcast(128), writes=["bfb"])
        p.dma("sp", tri[:], tri_d[:, :], writes=["tri"])
        p.op("dve", lambda e: e.memset(ssA[:], 0.0), writes=["ssA"])
        p.op("dve", lambda e: e.memset(onesf[:], 1.0), writes=["onesf"])

        pT = [ps[:, 0, :].bitcast(BF16), ps[:, 1, :].bitcast(BF16)]
        cnt_qk = [0]
        cnt_v = [0]
        cnt_ev = [0]
        winkeys = [("win", c) for c in range(KC)]

        for g in range(NG):
            tok0 = g * 512
            own = tok0 >= NPREV
            need_kd = own or tok0 >= KD0
            for tt in range(4):
                ti = 4 * g + tt
                xb = xt[ti % NXB]
                p.dma("sp", xb[:], xs[ti * 128:(ti + 1) * 128, :], writes=[("xt", ti % NXB)])
                p.op("act", lambda e, xb=xb, ti=ti: e.activation(junk[:], xb[:], AF.Square, accum_out=ssA[:, ti:ti + 1]),
                     reads=[("xt", ti % NXB), "ssA"], writes=["junk", ("ss", ti)])
            g4 = slice(4 * g, 4 * g + 4)
            p.op("dve", lambda e, g4=g4: e.tensor_scalar(rsA[:, g4], ssA[:, g4], 1.0 / D, EPS, ALU.mult, ALU.add),
                 reads=[("ss", 4 * g + i) for i in range(4)], writes=[("rs", g)])
            p.op("act", lambda e, g4=g4: e.activation(rsA[:, g4], rsA[:, g4], AF.Ln), reads=[("rs", g)], writes=[("rs", g)])
            p.op("act", lambda e, g4=g4: e.activation(rsA[:, g4], rsA[:, g4], AF.Exp, scale=-0.5), reads=[("rs", g)], writes=[("rs", g)])
            for tt in range(4):
                ti = 4 * g + tt
                xb = xt[ti % NXB]
                hbt = hb[ti % NHB]
                p.op("dve", lambda e, xb=xb, hbt=hbt, ti=ti: e.scalar_tensor_tensor(hbt[:], xb[:], rsA[:, ti:ti + 1], gbc[:], ALU.mult, ALU.mult),
                     reads=[("xt", ti % NXB), ("rs", g), "gbc"], writes=[("hb", ti % NHB)])
                pt = pT[ti % 2]
                for c in range(KC):
                    p.op("pe", lambda e, pt=pt, hbt=hbt, c=c: e.transpose(pt[:, c * 128:(c + 1) * 128], hbt[:, c * 128:(c + 1) * 128], identb[:]),
                         reads=[("hb", ti % NHB), "identb"], writes=[("pT", ti % 2)])
                dst = hT[g % 2][:, :, tt * 128:(tt + 1) * 128]
                src = pt.rearrange("p (c t) -> p c t", c=KC)
                if tt % 2 == 0:
                    p.op("act", lambda e, dst=dst, src=src: e.copy(dst, src), reads=[("pT", ti % 2)], writes=[("hT", g % 2, tt)])
                else:
                    p.op("dve", lambda e, dst=dst, src=src: e.tensor_copy(dst, src), reads=[("pT", ti % 2)], writes=[("hT", g % 2, tt)])
            hTk = [("hT", g % 2, tt) for tt in range(4)]
            blocks = []
            if own:
                o0 = tok0 - NPREV
                for b in range(4):
                    blocks.append((0 + b * 128, 0.125, QdT[b * 128:(b + 1) * 128, o0:o0 + 512]))
                for b in range(4):
                    blocks.append((1536 + b * 128, 0.125, QfT[b * 128:(b + 1) * 128, o0:o0 + 512]))
            if need_kd:
                k0 = tok0 - KD0
                for b in range(4):
                    blocks.append((512 + b * 128, 1.0, KdT[b * 128:(b + 1) * 128, k0:k0 + 512]))
            for b in range(4):
                blocks.append((2048 + b * 128, 1.0, KfT[b * 128:(b + 1) * 128, tok0:tok0 + 512]))
            for (col, scale, dst) in blocks:
                i = cnt_qk[0]
                cnt_qk[0] += 1
                pq = ps[:, 2 + i % 3, :]
                for c in range(KC):
                    p.op("pe", lambda e, pq=pq, c=c, col=col, g=g: e.matmul(pq, win[:, c, col:col + 128], hT[g % 2][:, c, :], start=(c == 0), stop=(c == KC - 1)),
                         reads=winkeys + hTk, writes=[("pQK", i % 3)])
                st = qst[i % 4]
                j = cnt_ev[0]
                cnt_ev[0] += 1
                if j % 2 == 0:
                    p.op("act", lambda e, st=st, pq=pq, scale=scale: e.activation(st[:], pq, AF.Copy, scale=scale),
                         reads=[("pQK", i % 3)], writes=[("qst", i % 4)])
                else:
                    p.op("dve", lambda e, st=st, pq=pq, scale=scale: e.tensor_scalar(st[:], pq, scale, None, ALU.mult),
                         reads=[("pQK", i % 3)], writes=[("qst", i % 4)])
                p.dma("pool", dst, st[:], reads=[("qst", i % 4)])
            for tt in range(4):
                ti = 4 * g + tt
                vlist = []
                if need_kd:
                    k0 = tok0 - KD0 + tt * 128
                    vlist.append((1024, VdS[k0:k0 + 128, :]))
                vlist.append((2560, VfS[ti * 128:(ti + 1) * 128, :]))
                for (col, dst) in vlist:
                    i = cnt_v[0]
                    cnt_v[0] += 1
                    pv = ps[:, 5 + i % 2, :]
                    for c in range(KC):
                        p.op("pe", lambda e, pv=pv, c=c, col=col, g=g, tt=tt: e.matmul(pv, hT[g % 2][:, c, tt * 128:(tt + 1) * 128], win[:, c, col:col + 512], start=(c == 0), stop=(c == KC - 1)),
                             reads=winkeys + [("hT", g % 2, tt)], writes=[("pV", i % 2)])
                    st = vst[i % 4]
                    j = cnt_ev[0]
                    cnt_ev[0] += 1
                    if j % 2 == 0:
                        p.op("act", lambda e, st=st, pv=pv: e.copy(st[:], pv), reads=[("pV", i % 2)], writes=[("vst", i % 4)])
                    else:
                        p.op("dve", lambda e, st=st, pv=pv: e.tensor_copy(st[:], pv), reads=[("pV", i % 2)], writes=[("vst", i % 4)])
                    p.dma("pool", dst, st[:], reads=[("vst", i % 4)])
                pf = ps[:, 7, (ti % 8) * 8:(ti % 8) * 8 + 8]
                for c in range(KC):
                    p.op("pe", lambda e, pf=pf, c=c, g=g, tt=tt: e.matmul(pf, hT[g % 2][:, c, tt * 128:(tt + 1) * 128], win[:, c, 3072:3080], start=(c == 0), stop=(c == KC - 1)),
                         reads=winkeys + [("hT", g % 2, tt)], writes=["pF7"])
                p.op("dve", lambda e, pf=pf, ti=ti: e.tensor_tensor(fall[:, ti, :], pf, bfb[:], ALU.add),
                     reads=["pF7", "bfb"], writes=["fall"])

        NF = NT * 8
        lall = sb.alloc("lall", [128, NF], F32)
        wcs = sb.alloc("wcs", [128, NF], F32)
        tA = sb.alloc("tA", [128, NF], F32)
        tB = sb.alloc("tB", [128, NF], F32)
        fflat = fall[:].rearrange("p t h -> p (t h)")
        p.op("act", lambda e: e.activation(lall[:], fflat, AF.Exp, scale=-1.0), reads=["fall"], writes=["lall"])
        p.op("act", lambda e: e.activation(lall[:], lall[:], AF.Ln, bias=1.0), reads=["lall"], writes=["lall"])
        nbk = (NF + 511) // 512
        assert nbk <= 2
        for bk in range(nbk):
            cs = slice(bk * 512, min(NF, (bk + 1) * 512))
            w = cs.stop - cs.start
            p.op("pe", lambda e, cs=cs, w=w, bk=bk: e.matmul(ps[:, 2 + bk, 0:w], tri[:], lall[:, cs], start=True, stop=True),
                 reads=["tri", "lall"], writes=[("pQK", bk)])
            p.op("pe", lambda e, cs=cs, w=w, bk=bk: e.matmul(ps[:, 5 + bk, 0:w], onesf[:], lall[:, cs], start=True, stop=True),
                 reads=["onesf", "lall"], writes=[("pV", bk)])
            p.op("dve", lambda e, cs=cs, w=w, bk=bk: e.tensor_copy(wcs[:, cs], ps[:, 2 + bk, 0:w]), reads=[("pQK", bk)], writes=["wcs"])
            p.op("act", lambda e, cs=cs, w=w, bk=bk: e.copy(tA[:, cs], ps[:, 5 + bk, 0:w]), reads=[("pV", bk)], writes=["tA"])
        src, dst = tA, tB
        k = 1
        while k < NT:
            kk = k * 8
            p.op("dve", lambda e, src=src, dst=dst, kk=kk: e.tensor_copy(dst[:, 0:kk], src[:, 0:kk]), reads=["tA", "tB"], writes=["tA", "tB"])
            p.op("dve", lambda e, src=src, dst=dst, kk=kk: e.tensor_tensor(dst[:, kk:NF], src[:, kk:NF], src[:, 0:NF - kk], ALU.add), reads=["tA", "tB"], writes=["tA", "tB"])
            src, dst = dst, src
            k *= 2
        incl = src
        tot = sb.alloc("tot", [128, NF], F32)
        for bk in range(nbk):
            cs = slice(bk * 512, min(NF, (bk + 1) * 512))
            w = cs.stop - cs.start
            p.op("act", lambda e, cs=cs, w=w, bk=bk: e.copy(tot[:, cs], ps[:, 5 + bk, 0:w]), reads=[("pV", bk)], writes=["tot"])
        cflat = ctok[:].rearrange("p t h -> p (t h)")
        p.op("dve", lambda e: e.tensor_tensor(wcs[:], wcs[:], incl[:], ALU.add), reads=["wcs", "tA", "tB"], writes=["wcs"])
        p.op("dve", lambda e: e.tensor_tensor(wcs[:], wcs[:], tot[:], ALU.subtract), reads=["wcs", "tot"], writes=["wcs"])
        p.op("dve", lambda e: e.tensor_scalar(cflat, wcs[:], -1.0, None, ALU.mult), reads=["wcs"], writes=["ctok"])
        p.dma("sp", cS.rearrange("(t p) h -> p t h", p=128), ctok[:], reads=["ctok"], writes=["cS"])
        if debug:
            finals.append(p.dma("sp", dbg["d_ctok"][:, :], cflat, reads=["ctok"]))
        p.dma("sp", crefbc[:], bass.AP(cS.tensor, NPREV * 8, [[0, 128], [1024 * 8, NQC], [1, 8]]),
              reads=["cS"], writes=["crefbc"])
        cT = sb.alloc("cT", [8, NOWN], F32)
        r1 = sb.alloc("r1", [8, NOWN], F32)
        aug = [sb.alloc(f"aug{i}", [8, NOWN], BF16) for i in range(3)]
        for t4 in range(NTO // 4):
            for i in range(4):
                ti = NTP + 4 * t4 + i
                p.op("pe", lambda e, ti=ti, i=i: e.transpose(ps[0:8, 2, i * 128:(i + 1) * 128], ctok[:, ti, :], identf[:]),
                     reads=["ctok", "identf"], writes=[("pQK", 0)])
            p.op("dve", lambda e, t4=t4: e.tensor_copy(cT[:, t4 * 512:(t4 + 1) * 512], ps[0:8, 2, :]), reads=[("pQK", 0)], writes=["cT"])
        for qc in range(NQC):
            qs = slice(qc * 1024, (qc + 1) * 1024)
            p.op("dve", lambda e, qs=qs: e.tensor_scalar(r1[:, qs], cT[:, qs], cT[:, qs.start:qs.start + 1], None, ALU.subtract),
                 reads=["cT"], writes=["r1"])
        for i in range(3):
            p.op("dve", lambda e, i=i: e.tensor_copy(aug[i][:], r1[:]), reads=["r1"], writes=[("aug", i)])
            if i < 2:
                p.op("dve", lambda e, i=i: e.tensor_tensor(r1[:], r1[:], aug[i][:], ALU.subtract), reads=["r1", ("aug", i)], writes=["r1"])
            p.dma("sp", QfA[:, i, :], aug[i][:], reads=[("aug", i)])

    phase_A()
    p.barrier()
    sb.reset(persist_mark)

    def phase_B():
        BM = sb.alloc("BM", [128, 24, 256], BF16)
        bt = sb.alloc("bt", [128, 24, 256], F32)
        msk = sb.alloc("msk", [128, 256], F32)
        neg = sb.alloc("neg", [128, 256], F32)
        p.dma("sp", bt[:].rearrange("p a b -> p (a b)"), btab_d[:, :], writes=["bt"])
        p.dma("sp", msk[:], bmsk_d[:, :], writes=["msk"])
        p.dma("sp", neg[:], bneg_d[:, :], writes=["neg"])
        for i in range(24):
            p.op("pool", lambda e, i=i: e.tensor_tensor(bt[:, i, :], bt[:, i, :], msk[:], ALU.mult), reads=["bt", "msk"], writes=["bt"])
            p.op("pool", lambda e, i=i: e.tensor_tensor(BM[:, i, :], bt[:, i, :], neg[:], ALU.add), reads=["bt", "neg"], writes=["BM"])
        kT = [sb.alloc(f"kT{i}", [128, 4096], BF16) for i in range(2)]
        qT = [sb.alloc(f"qT{i}", [128, 2048], BF16) for i in range(2)]
        q4 = [sb.alloc(f"q4{i}", [128, 2048], BF16) for i in range(2)]
        q16 = [sb.alloc(f"q16{i}", [128, 2048], BF16) for i in range(2)]
        vA = [[sb.alloc(f"vA{i}{b}", [128, 32, 128], BF16) for b in range(3)] for i in range(2)]
        Pb = [sb.alloc(f"Pb{i}", [128, 512], BF16) for i in range(4)]
        acc = sb.alloc("acc", [128, 2048], F32)
        rd0 = sb.alloc("rd0", [128, 2048], F32)
        onb = [sb.alloc(f"onb{i}", [128, 2048], BF16) for i in range(2)]
        Oflat = psb(0, 4)
        scnt = [0]
        pcnt = [0]

        steps = []
        for ch in range(NCH):
            for hp in range(4):
                for hh in range(2):
                    for br, dil in enumerate((1, 4, 16)):
                        kb_ = (ch * 4 + hp) % 2
                        qt, q4t, q16t = qT[kb_], q4[kb_], q16[kb_]
                        blk = []
                        if dil == 1:
                            for kb in range(-1, 16):
                                kc = slice(2048 + 128 * kb, 2048 + 128 * kb + 128)
                                if kb == -1:
                                    blk.append((kc, qt, slice(0, 128), 128, slice(128, 256), 16 + kb, slice(0, 128)))
                                elif kb == 15:
                                    blk.append((kc, qt, slice(1920, 2048), 128, slice(0, 128), 16 + kb, slice(1920, 2048)))
                                else:
                                    blk.append((kc, qt, slice(128 * kb, 128 * kb + 256), 256, slice(0, 256), 16 + kb, slice(128 * kb, 128 * kb + 256)))
                        elif dil == 4:
                            for r in range(4):
                                for g in range(-1, 4):
                                    kc = slice(2048 + 512 * g + r, 2048 + 512 * g + r + 509, 4)
                                    n = 4 * (g + 4) + r
                                    if g == -1:
                                        blk.append((kc, q4t, slice(r * 512, r * 512 + 128), 128, slice(128, 256), n, slice(r * 512, r * 512 + 128)))
                                    elif g == 3:
                                        blk.append((kc, q4t, slice(r * 512 + 384, r * 512 + 512), 128, slice(0, 128), n, slice(r * 512 + 384, r * 512 + 512)))
                                    else:
                                        blk.append((kc, q4t, slice(r * 512 + 128 * g, r * 512 + 128 * g + 256), 256, slice(0, 256), n, slice(r * 512 + 128 * g, r * 512 + 128 * g + 256)))
                        else:
                            for r in range(16):
                                for G in (-1, 0):
                                    kc = slice(2048 + 2048 * G + r, 2048 + 2048 * G + r + 2033, 16)
                                    n = 16 * (G + 1) + r
                                    blk.append((kc, q16t, slice(r * 128, r * 128 + 128), 128, slice(128, 256) if G == -1 else slice(0, 128), n, slice(r * 128, r * 128 + 128)))
                        npairs = (len(blk) + 1) // 2
                        for pi_ in range(npairs):
                            steps.append(dict(ch=ch, hp=hp, hh=hh, br=br, dil=dil, pair=blk[2 * pi_:2 * pi_ + 2],
                                              first_br=(pi_ == 0), last_br=(pi_ == npairs - 1)))
        for i, st in enumerate(steps):
            st["i"] = i
        touched = set()

        def pre_qk(st):
            ch, hp, hh, br = st["ch"], st["hp"], st["hh"], st["br"]
            if not (st["first_br"] and br == 0):
                return
            kb_ = (ch * 4 + hp) % 2
            if hh == 0:
                if hp == 0 and ch == 0:
                    for vb in range(2):
                        for b3 in range(3):
                            if ch == 0:
                                p.op("pool", lambda e, vb=vb, b3=b3: e.tensor_copy(vA[vb][b3][:, 0:16, 64:128], pvb[:].unsqueeze(1).broadcast_to([128, 16, 64])),
                                     reads=["pvb"], writes=[("vAc", vb, b3)])
                                p.op("pool", lambda e, vb=vb, b3=b3: e.memset(vA[vb][b3][:, 16:32, 64:128], 1.0), writes=[("vAc", vb, b3)])
                kt, qt, q4t, q16t = kT[kb_], qT[kb_], q4[kb_], q16[kb_]
                p.dma("sp", kt[:], KdT[hp * 128:(hp + 1) * 128, ch * 2048:ch * 2048 + 4096], writes=[("kT", kb_)])
                p.dma("sp", qt[:], QdT[hp * 128:(hp + 1) * 128, ch * 2048:(ch + 1) * 2048], writes=[("qT", kb_)])
                p.op("pool", lambda e, qt=qt, q4t=q4t: e.tensor_copy(q4t[:].rearrange("p (r g i) -> p r g i", r=4, g=4), qt[:].rearrange("p (g i r) -> p r g i", g=4, r=4)),
                     reads=[("qT", kb_)], writes=[("q4", kb_)])
                p.op("pool", lambda e, qt=qt, q16t=q16t: e.tensor_copy(q16t[:].rearrange("p (r i) -> p r i", r=16), qt[:].rearrange("p (i r) -> p r i", r=16)),
                     reads=[("qT", kb_)], writes=[("q16", kb_)])
            h = 2 * hp + hh
            vb = h % 2
            vsrc = VdS[ch * 2048:ch * 2048 + 4096, h * 64:(h + 1) * 64]
            for n4 in range(8):
                p.dma("sp", vA[vb][0][:, 4 * n4:4 * n4 + 4, 0:64], vsrc[n4 * 512:(n4 + 1) * 512, :].rearrange("(n i) c -> i n c", i=128), writes=[("vAv", vb, 0)])
            for g in range(8):
                p.dma("sp", vA[vb][1][:, 4 * g:4 * g + 4, 0:64], vsrc[g * 512:(g + 1) * 512, :].rearrange("(i r) c -> i r c", r=4), writes=[("vAv", vb, 1)])
            for G in range(2):
                v16 = vsrc[G * 2048:(G + 1) * 2048, :].rearrange("(i r) c -> i r c", r=16)
                for r4 in range(4):
                    p.dma("sp", vA[vb][2][:, 16 * G + 4 * r4:16 * G + 4 * r4 + 4, 0:64], v16[:, 4 * r4:4 * r4 + 4, :], writes=[("vAv", vb, 2)])

        def emit_qk(st):
            pre_qk(st)
            ch, hp, hh, br, dil, i = st["ch"], st["hp"], st["hh"], st["br"], st["dil"], st["i"]
            kb_ = (ch * 4 + hp) % 2
            kt = kT[kb_]
            h = 2 * hp + hh
            rows = slice(64 * hh, 64 * hh + 64)
            qkey = {1: ("qT", kb_), 4: ("q4", kb_), 16: ("q16", kb_)}[dil]
            bank = 4 + i % 4
            for slot, (kc, qsrc, qc_, N, bmc, n, oc) in enumerate(st["pair"]):
                so = ps[:, bank, slot * 256:slot * 256 + N]
                p.op("pe", lambda e, so=so, kc=kc, qsrc=qsrc, qc_=qc_, kt=kt, rows=rows: e.matmul(so, kt[rows, kc], qsrc[rows, qc_], start=True, stop=False),
                     reads=[("kT", kb_), qkey], writes=[("S", bank)])
                p.op("pe", lambda e, so=so, bmc=bmc, bi=br * 8 + h: e.matmul(so, identb[:], BM[:, bi, bmc], start=False, stop=True),
                     reads=["identb", "BM"], writes=[("S", bank)])

        def emit_rest(st):
            ch, hp, hh, br, dil, i = st["ch"], st["hp"], st["hh"], st["br"], st["dil"], st["i"]
            h = 2 * hp + hh
            vb = h % 2
            bank = 4 + i % 4
            pair = st["pair"]
            pb = Pb[i % 4]
            if len(pair) == 2 and pair[0][3] == 256 and pair[1][3] == 256:
                p.op("act", lambda e, pb=pb, bank=bank: e.activation(pb[:], ps[:, bank, :], AF.Exp), reads=[("S", bank)], writes=[("Pb", i % 4)])
            else:
                for slot, (kc, qsrc, qc_, N, bmc, n, oc) in enumerate(pair):
                    p.op("act", lambda e, pb=pb, bank=bank, slot=slot, N=N: e.activation(pb[:, slot * 256:slot * 256 + N], ps[:, bank, slot * 256:slot * 256 + N], AF.Exp),
                         reads=[("S", bank)], writes=[("Pb", i % 4)])
            if st["first_br"]:
                touched.clear()
            for slot, (kc, qsrc, qc_, N, bmc, n, oc) in enumerate(pair):
                if oc.start // 512 != (oc.stop - 1) // 512:
                    parts = [(oc.start, oc.start + 128, 0), (oc.start + 128, oc.stop, 128)]
                else:
                    parts = [(oc.start, oc.stop, 0)]
                for (o0, o1, po) in parts:
                    ob = o0 // 512
                    st_flag = ob not in touched
                    touched.add(ob)
                    p.op("pe", lambda e, pb=pb, slot=slot, n=n, o0=o0, o1=o1, po=po, vb=vb, br=br, st_flag=st_flag: e.matmul(Oflat[:, o0:o1], vA[vb][br][:, n, :], pb[:, slot * 256 + po:slot * 256 + po + (o1 - o0)], start=st_flag, stop=True, skip_group_check=True),
                         reads=[("Pb", i % 4), ("vAv", vb, br), ("vAc", vb, br)], writes=["O"])
            if st["last_br"]:
                if dil == 1:
                    p.op("dve", lambda e: e.tensor_copy(acc[:], Oflat), reads=["O"], writes=["acc"])
                elif dil == 4:
                    av = acc[:].rearrange("p (g i r) -> p r g i", g=4, r=4)
                    p.op("dve", lambda e, av=av: e.tensor_tensor(av, av, Oflat.rearrange("p (r g i) -> p r g i", r=4, g=4), ALU.add), reads=["O", "acc"], writes=["acc"])
                else:
                    av = acc[:].rearrange("p (i r) -> p r i", r=16)
                    p.op("dve", lambda e, av=av: e.tensor_tensor(av, av, Oflat.rearrange("p (r i) -> p r i", r=16), ALU.add), reads=["O", "acc"], writes=["acc"])
                    ob_ = onb[h % 2]
                    p.op("dve", lambda e: e.reciprocal(rd0[0:64, :], acc[64:128, :]), reads=["acc"], writes=["rd0"])
                    p.op("dve", lambda e, ob_=ob_: e.tensor_tensor(ob_[0:64, :], acc[0:64, :], rd0[0:64, :], ALU.mult), reads=["acc", "rd0"], writes=[("onb", h % 2)])
                    p.dma("pool", OcS[h * 64:(h + 1) * 64, ch * 2048:(ch + 1) * 2048], ob_[0:64, :], reads=[("onb", h % 2)])
                    if h == 7 and ch == 0 and NCH > 1:
                        for vb2 in range(2):
                            for b3 in range(3):
                                p.op("pool", lambda e, vb2=vb2, b3=b3: e.memset(vA[vb2][b3][:, 0:16, 64:128], 1.0), writes=[("vAc", vb2, b3)])

        if PIPE_B:
            emit_qk(steps[0])
            for i, st in enumerate(steps):
                if i + 1 < len(steps):
                    emit_qk(steps[i + 1])
                emit_rest(st)
        else:
            for i, st in enumerate(steps):
                emit_qk(st)
                emit_rest(st)

    phase_B()
    p.barrier()
    sb.reset(persist_mark)

    def phase_C():
        kT = [sb.alloc(f"kfT{i}", [128, NTOK], BF16) for i in range(2)]
        qT = [sb.alloc(f"qfT{i}", [128, NOWN], BF16) for i in range(2)]
        vA = [sb.alloc(f"vfA{i}", [128, NT, 128], BF16) for i in range(2)]
        biasq = [sb.alloc(f"biasq{i}", [128, NT], F32) for i in range(2)]
        Pb = [sb.alloc(f"Pf{i}", [128, 1024], BF16) for i in range(3)]
        rd0 = sb.alloc("rdf", [128, 1024], F32)
        onb = [sb.alloc(f"onf{i}", [128, 1024], BF16) for i in range(2)]
        for i in range(2):
            p.op("pool", lambda e, i=i: e.memset(kT[i][64:96, :], 1.0), writes=[("kfa", i)])
            p.op("pool", lambda e, i=i: e.tensor_copy(vA[i][:, 0:NTP, 64:128], pvb[:].unsqueeze(1).broadcast_to([128, NTP, 64])), reads=["pvb"], writes=[("vfc", i)])
            p.op("pool", lambda e, i=i: e.memset(vA[i][:, NTP:NT, 64:128], 1.0), writes=[("vfc", i)])
        kcnt = [0]
        wstage = [sb.alloc(f"wstage{i}", [128, 6144], BF16) for i in range(2)]

        def prep_expert(ex):
            b_ = ex % 2
            ws = wstage[b_]
            p.dma("pool", ws[:, 0:2048].rearrange("p (c n) -> p c n", c=KC), w_eg[ex].rearrange("(c p) n -> p c n", p=128), writes=[("wst", b_)])
            p.dma("pool", ws[:, 2048:4096].rearrange("p (c n) -> p c n", c=KC), w_eu[ex].rearrange("(c p) n -> p c n", p=128), writes=[("wst", b_)])
            p.dma("pool", ws[:, 4096:6144].rearrange("p (c n) -> p c n", c=2), w_ed[ex].rearrange("(c p) n -> p c n", p=128), writes=[("wst", b_)])
            p.dma("pool", WALL[ex * 128:(ex + 1) * 128, :], ws[:], reads=[("wst", b_)])

        def head_loads(h):
            hb_ = h % 2
            kt, qt, va = kT[hb_], qT[hb_], vA[hb_]
            p.dma("sp", kt[0:64, :], KfT[h * 64:(h + 1) * 64, :], writes=[("kf", hb_)])
            p.dma("sp", qt[0:64, :], QfT[h * 64:(h + 1) * 64, :], writes=[("qf", hb_)])
            p.dma("sp", qt[64:67, :], QfA[h, :, :], writes=[("qf", hb_)])
            for t8 in range(0, NT, 4):
                p.dma("sp", va[:, t8:t8 + 4, 0:64], VfS[t8 * 128:(t8 + 4) * 128, h * 64:(h + 1) * 64].rearrange("(n i) c -> i n c", i=128),
                      writes=[("vf", hb_)])

        steps = []
        oi = 0
        for h in range(8):
            for qc in range(NQC):
                nfull = (NPREV + 1024 * qc) // 128
                total = nfull + 8
                for kb in range(total):
                    steps.append(dict(h=h, qc=qc, kb=kb, nfull=nfull, total=total, oi=oi, first=(kb == 0), last=(kb == total - 1)))
                oi += 1
        for i, st in enumerate(steps):
            st["ki"] = i

        def halves(c0):
            res = []
            for half in range(2):
                lo, hi = max(c0, 512 * half), 512 * (half + 1)
                if lo < hi:
                    res.append((half, lo, hi))
            return res

        def emit_qk(st):
            h, qc, kb, ki = st["h"], st["qc"], st["kb"], st["ki"]
            hb_ = h % 2
            kt, qt = kT[hb_], qT[hb_]
            if st["first"]:
                if qc == 0 and h == 0:
                    head_loads(0)
                bq = biasq[st["oi"] % 2]
                total = st["total"]
                p.op("dve", lambda e, bq=bq, h=h, qc=qc, total=total: e.tensor_scalar(bq[:, 0:total], ctok[:, 0:total, h], -1.0, crefbc[:, qc, h:h + 1], ALU.mult, ALU.add),
                     reads=["ctok", "crefbc"], writes=[("biasq", st["oi"] % 2)])
            sb0 = 4 + 2 * (ki % 2)
            Sk = ("Sf", ki % 2)
            j = kb - st["nfull"]
            c0 = 128 * j if j >= 0 else 0
            for (half, lo, hi) in halves(c0):
                diag_here = (j >= 0 and lo == c0)
                p.op("pe", lambda e, sb0=sb0, half=half, lo=lo, hi=hi, kb=kb, qc=qc, kt=kt, qt=qt, diag_here=diag_here:
                     e.matmul(ps[:, sb0 + half, lo - 512 * half:512], kt[0:67, kb * 128:(kb + 1) * 128], qt[0:67, qc * 1024 + lo:qc * 1024 + hi], start=True, stop=not diag_here),
                     reads=[("kf", hb_), ("kfa", hb_), ("qf", hb_)], writes=[Sk])
                if diag_here:
                    p.op("pe", lambda e, sb0=sb0, half=half, lo=lo: e.matmul(ps[:, sb0 + half, lo - 512 * half:lo - 512 * half + 128], identb[:], mneg[:], start=False, stop=True),
                         reads=["identb", "mneg"], writes=[Sk])

        def emit_rest(st):
            h, qc, kb, ki, total = st["h"], st["qc"], st["kb"], st["ki"], st["total"]
            hb_ = h % 2
            va = vA[hb_]
            bq = biasq[st["oi"] % 2]
            bqk = ("biasq", st["oi"] % 2)
            sb0 = 4 + 2 * (ki % 2)
            Sk = ("Sf", ki % 2)
            pbi = ki % 3
            pb = Pb[pbi]
            ob0 = 2 * (st["oi"] % 2)
            Ok = ("Of", st["oi"] % 2)
            j = kb - st["nfull"]
            c0 = 128 * j if j >= 0 else 0
            Sflat = psb(sb0, 2)
            if st["first"] and qc == 0 and h + 1 < 8:
                head_loads(h + 1)
            p.op("act", lambda e, pb=pb, Sflat=Sflat, c0=c0, bq=bq, kb=kb: e.activation(pb[:, c0:1024], Sflat[:, c0:1024], AF.Exp, bias=bq[:, kb:kb + 1]),
                 reads=[Sk, bqk], writes=[("Pf", pbi)])
            for (half, lo, hi) in halves(c0):
                p.op("pe", lambda e, ob0=ob0, half=half, lo=lo, hi=hi, kb=kb, va=va, pb=pb, total=total:
                     e.matmul(ps[:, ob0 + half, lo - 512 * half:512], va[:, kb, :], pb[:, lo:hi], start=(kb == 0), stop=(kb == total - 1), skip_group_check=True),
                     reads=[("Pf", pbi), ("vf", hb_), ("vfc", hb_)], writes=[Ok])
            if st["last"]:
                Of = psb(ob0, 2)
                ob = onb[st["oi"] % 2]
                p.op("dve", lambda e, Of=Of: e.reciprocal(rd0[0:64, :], Of[64:128, :]), reads=[Ok], writes=["rdf"])
                p.op("dve", lambda e, Of=Of, ob=ob: e.tensor_tensor(ob[0:64, :], Of[0:64, :], rd0[0:64, :], ALU.mult), reads=[Ok, "rdf"], writes=[("onf", st["oi"] % 2)])
                p.dma("pool", OcS[512 + h * 64:512 + (h + 1) * 64, qc * 1024:(qc + 1) * 1024], ob[0:64, :], reads=[("onf", st["oi"] % 2)])

        prep_at = {}
        for ex in range(NEXP):
            prep_at.setdefault(min(len(steps) - 1, (ex * len(steps)) // NEXP), []).append(ex)
        if PIPE_C:
            emit_qk(steps[0])
            for i, st in enumerate(steps):
                if i + 1 < len(steps):
                    emit_qk(steps[i + 1])
                emit_rest(st)
                for ex in prep_at.get(i, []):
                    if not NO_WALL:
                        prep_expert(ex)
        else:
            for i, st in enumerate(steps):
                emit_qk(st)
                emit_rest(st)

    phase_C()
    p.barrier()
    sb.reset(persist_mark)

    def phase_D():
        T_ = NTO
        E_ = NEXP
        NTL = 2 * NOWN // 128 + 32
        NS = NTL * 128
        wo = sb.alloc("wo", [128, 8, D], BF16)
        gcat = sb.alloc("gcat", [128, 8], F32)
        wr = sb.alloc("wr", [128, KC, 36], BF16)
        brb = sb.alloc("brb", [128, 36], F32)
        g2bc = sb.alloc("g2bc", [128, D], F32)
        gfbc = sb.alloc("gfbc", [128, D], F32)
        tris = sb.alloc("tris", [128, 128], F32)
        onesf = sb.alloc("onesfD", [128, 128], F32)
        tokid = sb.alloc("tokid", [128, T_], F32)
        jv = sb.alloc("jv", [128, NTL], F32)
        pidx = sb.alloc("pidx", [128, 1], F32)
        Lg = sb.alloc("Lg", [128, T_, 36], F32)
        A1 = sb.alloc("A1", [128, T_, E_], F32)
        A2 = sb.alloc("A2", [128, T_, E_], F32)
        wk = sb.alloc("wk", [128, T_, 2], F32)
        widx = sb.alloc("widx", [128, NTL], I32)
        junk = sb.alloc("junkD", [128, D], BF16)
        mX = sb.mark()
        wof = sb.alloc("wof", [128, 8, D], F32)
        p.dma("sp", wof[:], w_out.rearrange("(b p) n -> p b n", p=128), writes=["wof"])
        p.dma("sp", gcat[:, 0:4], out_norm_dil.rearrange("(b p) -> p b", p=128), writes=["gcat"], allow_slow_non_contiguous=True)
        p.dma("sp", gcat[:, 4:8], out_norm_fox.rearrange("(b p) -> p b", p=128), writes=["gcat"], allow_slow_non_contiguous=True)
        for b in range(8):
            eng = "dve" if b % 2 == 0 else "pool"
            p.op(eng, lambda e, b=b: e.tensor_scalar(wo[:, b, :], wof[:, b, :], gcat[:, b:b + 1], None, ALU.mult), reads=["wof", "gcat"], writes=[("wo", b)])
        p.dma("pool", wr[:, :, 0:4], w_rg.rearrange("(c p) n -> p c n", p=128), writes=["wr"])
        p.dma("pool", wr[:, :, 4:36], w_re.rearrange("(c p) n -> p c n", p=128), writes=["wr"])
        p.dma("sp", brb[:, 0:4], b_rg.partition_broadcast(128), writes=["brb"])
        p.dma("sp", brb[:, 4:36], b_re.partition_broadcast(128), writes=["brb"])
        p.dma("sp", g2bc[:], ffn_norm.partition_broadcast(128), writes=["g2bc"])
        p.dma("sp", gfbc[:], final_norm.partition_broadcast(128), writes=["gfbc"])
        p.dma("sp", tris[:], tris_d[:, :], writes=["tris"])
        p.dma("sp", tokid[:], tokid_d[:, :], writes=["tokid"])
        p.dma("sp", jv[:], jv_d[:, :], writes=["jv"])
        p.dma("sp", pidx[:], pidx_d[:, :], writes=["pidx"])
        p.dma("sp", SLOTINFO.rearrange("(p r) c -> p (r c)", p=128), slot_init_d.rearrange("(p r) c -> p (r c)", p=128), writes=["slotinfo"])
        p.op("pool", lambda e: e.memset(onesf[:], 1.0), writes=["onesfD"])

        OcT = [sb.alloc(f"OcT{i}", [128, 8, 512], BF16) for i in range(2)]
        xo = [sb.alloc(f"xo{i}", [128, D], F32) for i in range(3)]
        NX1 = 6
        x1 = [sb.alloc(f"x1{i}", [128, D], F32) for i in range(NX1)]
        sq8 = [sb.alloc(f"sq8{i}", [128, 8, 128], BF16) for i in range(2)]
        h2b = [sb.alloc(f"h2b{i}", [128, D], BF16) for i in range(3)]
        h2T = [sb.alloc(f"h2T{i}", [128, KC, 128], BF16) for i in range(2)]
        st1 = sb.alloc("st1", [128, T_, 2], F32)
        st2 = sb.alloc("st2", [128, T_], F32)
        p.op("dve", lambda e: e.memset(st2[:], 0.0), writes=["st2z"])
        for g4 in range(T_ // 4):
            oc = OcT[g4 % 2]
            ock = ("OcT", g4 % 2)
            p.dma("sp", oc[:], OcS[:, g4 * 512:(g4 + 1) * 512].rearrange("(b p) t -> p b t", p=128), writes=[ock])
            for i in range(4):
                tt = 4 * g4 + i
                ts_ = slice(i * 128, (i + 1) * 128)
                s8 = sq8[tt % 2]
                p.op("pool", lambda e, s8=s8, oc=oc, ts_=ts_: e.tensor_tensor(s8[:], oc[:, :, ts_], oc[:, :, ts_], ALU.mult), reads=[ock], writes=[("sq8", tt % 2)])
                for grp in range(2):
                    for b in range(4):
                        p.op("pe", lambda e, s8=s8, grp=grp, b=b, i=i: e.matmul(ps[:, 7, 2 * i + grp:2 * i + grp + 1], s8[:, 4 * grp + b, :], ones_bf[:], start=(b == 0), stop=(b == 3), skip_group_check=True),
                             reads=[("sq8", tt % 2), "ones_bf"], writes=[("bk", 7)])
            gs = slice(4 * g4, 4 * g4 + 4)
            st1g = st1[:, gs, :].rearrange("p t k -> p (t k)")
            p.op("dve", lambda e, st1g=st1g: e.tensor_scalar(st1g, ps[:, 7, 0:8], 1.0 / 512, EPS, ALU.mult, ALU.add), reads=[("bk", 7)], writes=[("st1", g4)])
            p.op("act", lambda e, st1g=st1g: e.activation(st1g, st1g, AF.Ln), reads=[("st1", g4)], writes=[("st1", g4)])
            p.op("act", lambda e, st1g=st1g: e.activation(st1g, st1g, AF.Exp, scale=-0.5), reads=[("st1", g4)], writes=[("st1", g4)])
            for i in range(4):
                tt = 4 * g4 + i
                ts_ = slice(i * 128, (i + 1) * 128)
                xb = xo[tt % 3]
                x1t = x1[tt % NX1]
                p.dma("sp", xb[:], xs[NPREV + tt * 128:NPREV + (tt + 1) * 128, :], writes=[("xo", tt % 3)])
                for grp in range(2):
                    for half in range(2):
                        for b in range(4):
                            p.op("pe", lambda e, grp=grp, half=half, b=b, ts_=ts_, oc=oc: e.matmul(ps[:, 2 * grp + half, :], oc[:, 4 * grp + b, ts_], wo[:, 4 * grp + b, half * 512:(half + 1) * 512], start=(b == 0), stop=(b == 3)),
                                 reads=[ock, ("wo", 4 * grp + b)], writes=[("bk", 2 * grp + half)])
                p.op("dve", lambda e, x1t=x1t, xb=xb, tt=tt: e.scalar_tensor_tensor(x1t[:], psb(0, 2), st1[:, tt, 0:1], xb[:], ALU.mult, ALU.add),
                     reads=[("bk", 0), ("bk", 1), ("st1", g4), ("xo", tt % 3)], writes=[("x1", tt % NX1)])
                p.op("dve", lambda e, x1t=x1t, tt=tt: e.scalar_tensor_tensor(x1t[:], psb(2, 2), st1[:, tt, 1:2], x1t[:], ALU.mult, ALU.add),
                     reads=[("bk", 2), ("bk", 3), ("st1", g4), ("x1", tt % NX1)], writes=[("x1", tt % NX1)])
                p.dma("pool", X1S[tt * 128:(tt + 1) * 128, :], x1t[:], reads=[("x1", tt % NX1)])
                if debug:
                    finals.append(p.dma("pool", dbg["d_x1"][tt * 128:(tt + 1) * 128, :], x1t[:], reads=[("x1", tt % NX1)]))
                p.op("act", lambda e, x1t=x1t, tt=tt: e.activation(junk[:], x1t[:], AF.Square, accum_out=st2[:, tt:tt + 1]), reads=[("x1", tt % NX1), "st2z"], writes=["junkD", ("ss2", tt)])
            st2g = st2[:, gs]
            p.op("dve", lambda e, st2g=st2g: e.tensor_scalar(st2g, st2g, 1.0 / D, EPS, ALU.mult, ALU.add), reads=[("ss2", 4 * g4 + i) for i in range(4)], writes=[("st2", g4)])
            p.op("act", lambda e, st2g=st2g: e.activation(st2g, st2g, AF.Ln), reads=[("st2", g4)], writes=[("st2", g4)])
            p.op("act", lambda e, st2g=st2g: e.activation(st2g, st2g, AF.Exp, scale=-0.5), reads=[("st2", g4)], writes=[("st2", g4)])
            for i in range(4):
                tt = 4 * g4 + i
                x1t = x1[tt % NX1]
                hbt = h2b[tt % 3]
                p.op("dve", lambda e, hbt=hbt, x1t=x1t, tt=tt: e.scalar_tensor_tensor(hbt[:], x1t[:], st2[:, tt:tt + 1], g2bc[:], ALU.mult, ALU.mult),
                     reads=[("x1", tt % NX1), ("st2", g4), "g2bc"], writes=[("h2b", tt % 3)])
                p.dma("pool", H2S[tt * 128:(tt + 1) * 128, :], hbt[:], reads=[("h2b", tt % 3)])
                pt = ps[:, 4 + tt % 2, :].bitcast(BF16)
                for c in range(KC):
                    p.op("pe", lambda e, pt=pt, hbt=hbt, c=c: e.transpose(pt[:, c * 128:(c + 1) * 128], hbt[:, c * 128:(c + 1) * 128], identb[:]),
                         reads=[("h2b", tt % 3), "identb"], writes=[("bk", 4 + tt % 2)])
                hT = h2T[tt % 2]
                p.op("act", lambda e, pt=pt, hT=hT: e.copy(hT[:], pt.rearrange("p (c t) -> p c t", c=KC)), reads=[("bk", 4 + tt % 2)], writes=[("h2T", tt % 2)])
                lp = ps[:, 6, (tt % 8) * 36:(tt % 8) * 36 + 36]
                for c in range(KC):
                    p.op("pe", lambda e, c=c, hT=hT, lp=lp: e.matmul(lp, hT[:, c, :], wr[:, c, :], start=(c == 0), stop=(c == KC - 1), skip_group_check=True),
                         reads=[("h2T", tt % 2), "wr"], writes=[("bk", 6)])
                p.op("dve", lambda e, lp=lp, tt=tt: e.tensor_tensor(Lg[:, tt, :], lp, brb[:], ALU.add), reads=[("bk", 6), "brb"], writes=["Lg"])

        if SKIP_D1B:
            p.barrier()
            return
        rk = "Lg"
        R = sb.alloc("Rr", [128, T_, 16], F32)
        Lm = sb.alloc("Lm", [128, T_, E_], F32)
        tmpE = sb.alloc("tmpE", [128, T_, E_], F32)

        def bc(ap2, n):
            return ap2.unsqueeze(2).broadcast_to([128, T_, n])

        Lgrp = Lg[:, :, 0:4]
        Lexp = Lg[:, :, 4:36]
        p.op("dve", lambda e: e.tensor_reduce(R[:, :, 0], Lgrp, AX.X, ALU.max), reads=[rk], writes=[rk])
        p.op("dve", lambda e: e.tensor_tensor(R[:, :, 4:8], Lgrp, bc(R[:, :, 0], 4), ALU.subtract), reads=[rk], writes=[rk])
        p.op("act", lambda e: e.activation(R[:, :, 8:12], R[:, :, 4:8], AF.Exp), reads=[rk], writes=[rk])
        p.op("dve", lambda e: e.tensor_reduce(R[:, :, 1], R[:, :, 8:12], AX.X, ALU.add), reads=[rk], writes=[rk])
        p.op("dve", lambda e: e.reciprocal(R[:, :, 1], R[:, :, 1]), reads=[rk], writes=[rk])
        p.op("dve", lambda e: e.tensor_tensor(R[:, :, 4:8], Lgrp, bc(R[:, :, 0], 4), ALU.is_equal), reads=[rk], writes=[rk])
        p.op("dve", lambda e: e.tensor_scalar(R[:, :, 4:8], R[:, :, 4:8], 1.0, 1e30, ALU.subtract, ALU.mult), reads=[rk], writes=[rk])
        p.op("dve", lambda e: e.tensor_tensor(Lm[:].rearrange("p t (g j) -> p t g j", g=4), Lexp.rearrange("p t (g j) -> p t g j", g=4),
                                              R[:, :, 4:8].unsqueeze(3).broadcast_to([128, T_, 4, 8]), ALU.add), reads=[rk], writes=[rk])
        p.op("dve", lambda e: e.tensor_reduce(R[:, :, 2], Lm[:], AX.X, ALU.max), reads=[rk], writes=[rk])
        p.op("dve", lambda e: e.tensor_tensor(A1[:], Lm[:], bc(R[:, :, 2], E_), ALU.is_equal), reads=[rk], writes=[rk])
        p.op("dve", lambda e: e.scalar_tensor_tensor(Lm[:], A1[:], -1e30, Lm[:], ALU.mult, ALU.add), reads=[rk], writes=[rk])
        p.op("dve", lambda e: e.tensor_reduce(R[:, :, 3], Lm[:], AX.X, ALU.max), reads=[rk], writes=[rk])
        p.op("dve", lambda e: e.tensor_tensor(A2[:], Lm[:], bc(R[:, :, 3], E_), ALU.is_equal), reads=[rk], writes=[rk])
        p.op("dve", lambda e: e.tensor_tensor(R[:, :, 12], R[:, :, 3], R[:, :, 2], ALU.subtract), reads=[rk], writes=[rk])
        p.op("act", lambda e: e.activation(R[:, :, 12], R[:, :, 12], AF.Exp), reads=[rk], writes=[rk])
        p.op("dve", lambda e: e.tensor_scalar(R[:, :, 13], R[:, :, 12], 1.0, None, ALU.add), reads=[rk], writes=[rk])
        p.op("dve", lambda e: e.reciprocal(R[:, :, 13], R[:, :, 13]), reads=[rk], writes=[rk])
        p.op("dve", lambda e: e.tensor_tensor(R[:, :, 14], R[:, :, 12], R[:, :, 13], ALU.mult), reads=[rk], writes=[rk])
        p.op("dve", lambda e: e.tensor_tensor(wk[:, :, 0], R[:, :, 13], R[:, :, 1], ALU.mult), reads=[rk], writes=[rk])
        p.op("dve", lambda e: e.tensor_tensor(wk[:, :, 1], R[:, :, 14], R[:, :, 1], ALU.mult), reads=[rk], writes=[rk])
        if debug:
            cmb = sb.alloc("cmb", [128, T_, E_], F32)
            p.op("dve", lambda e: e.tensor_tensor(cmb[:], A1[:], bc(wk[:, :, 0], E_), ALU.mult), reads=[rk], writes=["cmb"])
            p.op("dve", lambda e: e.tensor_tensor(tmpE[:], A2[:], bc(wk[:, :, 1], E_), ALU.mult), reads=[rk], writes=["tmpE"])
            p.op("dve", lambda e: e.tensor_tensor(cmb[:], cmb[:], tmpE[:], ALU.add), reads=["cmb", "tmpE"], writes=["cmb"])
            finals.append(p.dma("sp", dbg["d_comb"].rearrange("(t p) e -> p t e", p=128), cmb[:], reads=["cmb"]))

        if SKIP_D2:
            p.barrier()
            return
        NF = T_ * E_
        Aall = sb.alloc("Aall", [128, NF], F32)
        Wsb = sb.alloc("Wsb", [128, NF], F32)
        tA = sb.alloc("tA2", [128, NF], F32)
        tB = sb.alloc("tB2", [128, NF], F32)
        Tot = sb.alloc("Tot", [128, NF], F32)
        A1f = A1[:].rearrange("p t e -> p (t e)")
        A2f = A2[:].rearrange("p t e -> p (t e)")
        p.op("dve", lambda e: e.tensor_tensor(Aall[:], A1f, A2f, ALU.add), reads=[rk], writes=["Aall"])
        nbk = NF // 512
        assert nbk <= 2
        for bk in range(nbk):
            cs = slice(bk * 512, (bk + 1) * 512)
            p.op("pe", lambda e, cs=cs, bk=bk: e.matmul(ps[:, bk, :], tris[:], Aall[:, cs], start=True, stop=True), reads=["tris", "Aall"], writes=[("bk", bk)])
            p.op("pe", lambda e, cs=cs, bk=bk: e.matmul(ps[:, 2 + bk, :], onesf[:], Aall[:, cs], start=True, stop=True), reads=["onesfD", "Aall"], writes=[("bk", 2 + bk)])
            p.op("dve", lambda e, cs=cs, bk=bk: e.tensor_copy(Wsb[:, cs], ps[:, bk, :]), reads=[("bk", bk)], writes=["Wsb"])
            p.op("act", lambda e, cs=cs, bk=bk: e.copy(tA[:, cs], ps[:, 2 + bk, :]), reads=[("bk", 2 + bk)], writes=["tAB"])
            p.op("act", lambda e, cs=cs, bk=bk: e.copy(Tot[:, cs], ps[:, 2 + bk, :]), reads=[("bk", 2 + bk)], writes=["Tot"])
        src, dst = tA, tB
        k = 1
        while k < T_:
            kk = k * E_
            p.op("dve", lambda e, src=src, dst=dst, kk=kk: e.tensor_copy(dst[:, 0:kk], src[:, 0:kk]), reads=["tAB"], writes=["tAB"])
            p.op("dve", lambda e, src=src, dst=dst, kk=kk: e.tensor_tensor(dst[:, kk:NF], src[:, kk:NF], src[:, 0:NF - kk], ALU.add), reads=["tAB"], writes=["tAB"])
            src, dst = dst, src
            k *= 2
        if D2_STOP == 1:
            p.barrier()
            return
        incl = src
        cnt_f = sb.alloc("cnt_f", [128, E_], F32)
        cnt_i = sb.alloc("cnt_i", [128, E_], I32)
        psz = sb.alloc("psz", [128, E_], F32)
        eA = sb.alloc("eA", [128, E_], F32)
        eB = sb.alloc("eB", [128, E_], F32)
        base = sb.alloc("base", [128, E_], F32)
        p.op("dve", lambda e: e.tensor_copy(cnt_f[:], incl[:, NF - E_:NF]), reads=["tAB"], writes=["cnt"])
        p.op("dve", lambda e: e.tensor_copy(cnt_i[:], cnt_f[:]), reads=["cnt"], writes=["cnti"])
        p.op("dve", lambda e: e.tensor_scalar(cnt_i[:], cnt_i[:], 127, None, ALU.add), reads=["cnti"], writes=["cnti"])
        p.op("dve", lambda e: e.tensor_scalar(cnt_i[:], cnt_i[:], 7, 7, ALU.arith_shift_right, ALU.logical_shift_left), reads=["cnti"], writes=["cnti"])
        p.op("dve", lambda e: e.tensor_copy(psz[:], cnt_i[:]), reads=["cnti"], writes=["psz"])
        if D2_STOP == 2:
            p.barrier()
            return
        p.op("dve", lambda e: e.tensor_copy(eA[:], psz[:]), reads=["psz"], writes=["eAB"])
        src2, dst2 = eA, eB
        k = 1
        HS_MAX = int(_os.environ.get('HS_MAX', '64'))
        while k < min(E_, HS_MAX):
            p.op("dve", lambda e, src2=src2, dst2=dst2, k=k: e.tensor_copy(dst2[:, 0:k], src2[:, 0:k]), reads=["eAB"], writes=["eAB"])
            p.op("dve", lambda e, src2=src2, dst2=dst2, k=k: e.tensor_tensor(dst2[:, k:E_], src2[:, k:E_], src2[:, 0:E_ - k], ALU.add), reads=["eAB"], writes=["eAB"])
            src2, dst2 = dst2, src2
            k *= 2
        endp = src2
        p.op("dve", lambda e: e.tensor_tensor(base[:], endp[:], psz[:], ALU.subtract), reads=["eAB", "psz"], writes=["base"])
        if D2_STOP == 3:
            p.barrier()
            return
        p.op("dve", lambda e: e.tensor_tensor(Wsb[:], Wsb[:], incl[:], ALU.add), reads=["Wsb", "tAB"], writes=["Wsb"])
        p.op("dve", lambda e: e.tensor_tensor(Wsb[:], Wsb[:], Tot[:], ALU.subtract), reads=["Wsb", "Tot"], writes=["Wsb"])
        W3 = Wsb[:].rearrange("p (t e) -> p t e", e=E_)
        p.op("dve", lambda e: e.tensor_tensor(W3, W3, base[:].unsqueeze(1).broadcast_to([128, T_, E_]), ALU.add), reads=["Wsb", "base"], writes=["Wsb"])
        slotf = sb.alloc("slotf", [128, T_, 2], F32)
        sl_i = sb.alloc("sl_i", [128, T_, 2], I32)
        for k_, Ak in enumerate((A1, A2)):
            p.op("dve", lambda e, Ak=Ak: e.tensor_tensor(tmpE[:], W3, Ak[:], ALU.mult), reads=["Wsb", rk], writes=["tmpE"])
            p.op("dve", lambda e, k_=k_: e.tensor_reduce(slotf[:, :, k_], tmpE[:], AX.X, ALU.add), reads=["tmpE"], writes=["slotf"])
        p.op("dve", lambda e: e.tensor_copy(sl_i[:], slotf[:]), reads=["slotf"], writes=["sl_i"])
        if debug:
            finals.append(p.dma("sp", dbg["d_slot"][:, :], slotf[:].rearrange("p t k -> p (t k)"), reads=["slotf"], writes=["dslot"]))
            finals.append(p.dma("sp", dbg["d_cnt"][:, 0:32], cnt_f[:], reads=["cnt"]))
            finals.append(p.dma("sp", dbg["d_cnt"][:, 32:64], psz[:], reads=["psz"]))
            finals.append(p.dma("sp", dbg["d_cnt"][:, 64:96], base[:], reads=["base"]))
        if D2_STOP == 4:
            p.barrier()
            return
        info = sb.alloc("info", [128, T_, 2, 4], I32)
        info_f = info[:].bitcast(F32)
        p.op("dve", lambda e: e.memset(info[:], 0), writes=["info"])
        for k_ in range(2):
            p.op("dve", lambda e, k_=k_: e.tensor_copy(info[:, :, k_, 0], tokid[:]), reads=["tokid", "info"], writes=["info"])
            p.op("dve", lambda e, k_=k_: e.tensor_scalar(slotf[:, :, k_], tokid[:], float(k_ * NOWN), None, ALU.add), reads=["tokid", "sl_i", "dslot"], writes=["slotf2"])
            p.op("dve", lambda e, k_=k_: e.tensor_copy(info[:, :, k_, 1], slotf[:, :, k_]), reads=["slotf2", "info"], writes=["info"])
            p.op("dve", lambda e, k_=k_: e.tensor_copy(info_f[:, :, k_, 2], wk[:, :, k_]), reads=[rk, "info"], writes=["info"])
        if not SKIP_D3:
            for tt in range(T_):
                for k_ in range(2):
                    p.dma_custom("pool", lambda e, tt=tt, k_=k_: e.indirect_dma_start(out=SLOTINFO[:, :], out_offset=bass.IndirectOffsetOnAxis(ap=sl_i[:, tt, k_:k_ + 1], axis=0),
                                                                                         in_=info[:, tt, k_, :], in_offset=None),
                                 reads=["info", "sl_i", "slotinfo"], writes=[("slotw", tt, k_)])
        if D2_STOP == 5:
            p.barrier()
            return
        cmpT = sb.alloc("cmpT", [128, NTL, E_], F32)
        eidf = sb.alloc("eidf", [128, NTL], F32)
        p.op("dve", lambda e: e.tensor_tensor(cmpT[:], endp[:].unsqueeze(1).broadcast_to([128, NTL, E_]), jv[:].unsqueeze(2).broadcast_to([128, NTL, E_]), ALU.is_le),
             reads=["eAB", "jv"], writes=["cmpT"])
        p.op("dve", lambda e: e.tensor_reduce(eidf[:], cmpT[:], AX.X, ALU.add), reads=["cmpT"], writes=["eidf"])
        p.op("dve", lambda e: e.tensor_scalar(eidf[:], eidf[:], float(E_ - 1), 128.0, ALU.min, ALU.mult), reads=["eidf"], writes=["eidf"])
        p.op("dve", lambda e: e.tensor_scalar(eidf[:], eidf[:], pidx[:, 0:1], None, ALU.add), reads=["eidf", "pidx"], writes=["eidf"])
        p.op("dve", lambda e: e.tensor_copy(widx[:], eidf[:]), reads=["eidf"], writes=["widx"])
        if debug:
            finals.append(p.dma("sp", dbg["d_eid"][:, :], eidf[:], reads=["eidf"]))
        if SKIP_D3:
            p.barrier()
            return
        p.barrier()

        sb.reset(mX)
        NB3 = 6
        LOOK = 3
        infoj = [sb.alloc(f"infoj{i}", [128, 4], I32) for i in range(NB3)]
        hg = [sb.alloc(f"hg{i}", [128, D], BF16) for i in range(NB3)]
        wt = [sb.alloc(f"wt{i}", [128, 6144], BF16) for i in range(NB3)]
        hgT = [sb.alloc(f"hgT{i}", [128, KC, 128], BF16) for i in range(2)]
        sg = [sb.alloc(f"sg{i}", [128, DEXP], F32) for i in range(2)]
        he = [sb.alloc(f"he{i}", [128, DEXP], BF16) for i in range(3)]
        heT = [sb.alloc(f"heT{i}", [128, 2, 128], BF16) for i in range(2)]
        ysb = [sb.alloc(f"ysb{i}", [128, D], F32) for i in range(2)]
        ycnt = [0]

        def t_load(j):
            b3 = j % NB3
            p.dma("sp", infoj[b3][:], SLOTINFO[j * 128:(j + 1) * 128, :], writes=[("infoj", b3)])
            p.dma_custom("pool", lambda e, b3=b3: e.indirect_dma_start(out=hg[b3][:], out_offset=None, in_=H2S[:, :], in_offset=bass.IndirectOffsetOnAxis(ap=infoj[b3][:, 0:1], axis=0)),
                         reads=[("infoj", b3)], writes=[("hg", b3)])
            p.dma_custom("pool", lambda e, b3=b3, j=j: e.indirect_dma_start(out=wt[b3][:], out_offset=None, in_=WALL[:, :], in_offset=bass.IndirectOffsetOnAxis(ap=widx[:, j:j + 1], axis=0)),
                         reads=["widx"], writes=[("wt", b3)])

        def st_T(j):
            b3, b2 = j % NB3, j % 2
            pt = ps[:, b2, :].bitcast(BF16)
            for c in range(KC):
                p.op("pe", lambda e, pt=pt, c=c, b3=b3: e.transpose(pt[:, c * 128:(c + 1) * 128], hg[b3][:, c * 128:(c + 1) * 128], identb[:]),
                     reads=[("hg", b3), "identb"], writes=[("bk", b2)])
            p.op("act", lambda e, pt=pt, b2=b2: e.copy(hgT[b2][:], pt.rearrange("p (c t) -> p c t", c=KC)), reads=[("bk", b2)], writes=[("hgT", b2)])

        def st_G(j):
            b3, b2, h3 = j % NB3, j % 2, j % 3
            gu = ps[:, 2 + b2, :]
            for w0 in (0, 2048):
                for c in range(KC):
                    p.op("pe", lambda e, gu=gu, w0=w0, c=c, b3=b3, b2=b2: e.matmul(gu[:, (w0 // 2048) * 256:(w0 // 2048) * 256 + 256], hgT[b2][:, c, :], wt[b3][:, w0 + c * 256:w0 + (c + 1) * 256], start=(c == 0), stop=(c == KC - 1), skip_group_check=True),
                         reads=[("hgT", b2), ("wt", b3)], writes=[("bk", 2 + b2)])
            p.op("act", lambda e, gu=gu, b2=b2: e.activation(sg[b2][:], gu[:, 0:256], AF.Silu), reads=[("bk", 2 + b2)], writes=[("sg", b2)])
            wcol = infoj[b3][:].bitcast(F32)[:, 2:3]
            p.op("dve", lambda e, gu=gu, b2=b2, h3=h3, wcol=wcol: e.scalar_tensor_tensor(he[h3][:], gu[:, 256:512], wcol, sg[b2][:], ALU.mult, ALU.mult),
                 reads=[("bk", 2 + b2), ("sg", b2), ("infoj", b3)], writes=[("he", h3)])

        def st_H(j):
            b2, h3 = j % 2, j % 3
            pt = ps[:, 4, :].bitcast(BF16)
            for k_ in range(2):
                p.op("pe", lambda e, pt=pt, k_=k_, h3=h3: e.transpose(pt[:, k_ * 128:(k_ + 1) * 128], he[h3][:, k_ * 128:(k_ + 1) * 128], identb[:]),
                     reads=[("he", h3), "identb"], writes=[("bk", 4)])
            p.op("act", lambda e, pt=pt, b2=b2: e.copy(heT[b2][:], pt[:, 0:256].rearrange("p (k t) -> p k t", k=2)), reads=[("bk", 4)], writes=[("heT", b2)])

        def st_Y(j):
            b3, b2 = j % NB3, j % 2
            for half in range(2):
                yb = 5 + ycnt[0] % 3
                ycnt[0] += 1
                for k_ in range(2):
                    p.op("pe", lambda e, yb=yb, k_=k_, half=half, b2=b2, b3=b3: e.matmul(ps[:, yb, :], heT[b2][:, k_, :], wt[b3][:, 4096 + k_ * 1024 + half * 512:4096 + k_ * 1024 + (half + 1) * 512], start=(k_ == 0), stop=(k_ == 1)),
                         reads=[("heT", b2), ("wt", b3)], writes=[("bk", yb)])
                if half == 0:
                    p.op("act", lambda e, yb=yb, b2=b2: e.copy(ysb[b2][:, 0:512], ps[:, yb, :]), reads=[("bk", yb)], writes=[("ysb", b2, 0)])
                else:
                    p.op("dve", lambda e, yb=yb, b2=b2: e.tensor_copy(ysb[b2][:, 512:1024], ps[:, yb, :]), reads=[("bk", yb)], writes=[("ysb", b2, 1)])
            p.dma_custom("pool", lambda e, b2=b2, b3=b3: e.indirect_dma_start(out=YS[:, :], out_offset=bass.IndirectOffsetOnAxis(ap=infoj[b3][:, 1:2], axis=0), in_=ysb[b2][:], in_offset=None),
                         reads=[("ysb", b2, 0), ("ysb", b2, 1), ("infoj", b3)])

        for j in range(min(LOOK, NTL)):
            t_load(j)
        for i in range(NTL + 3):
            if i < NTL:
                st_T(i)
            if 0 <= i - 1 < NTL:
                st_G(i - 1)
            if 0 <= i - 2 < NTL:
                st_H(i - 2)
            if 0 <= i - 3 < NTL:
                st_Y(i - 3)
            if i + LOOK < NTL:
                t_load(i + LOOK)
        p.barrier()

        sb.reset(mX)
        xa = [sb.alloc(f"xa{i}", [128, D], F32) for i in range(6)]
        ya = [sb.alloc(f"ya{i}", [128, D], F32) for i in range(3)]
        yb_ = [sb.alloc(f"yb{i}", [128, D], F32) for i in range(3)]
        outt = [sb.alloc(f"outt{i}", [128, D], F32) for i in range(3)]
        st3 = sb.alloc("st3", [128, T_], F32)
        p.op("dve", lambda e: e.memset(st3[:], 0.0), writes=["st3z"])
        for g4 in range(T_ // 4):
            for i in range(4):
                tt = 4 * g4 + i
                b3 = tt % 3
                b6 = tt % 6
                rs = slice(tt * 128, (tt + 1) * 128)
                p.dma("sp", xa[b6][:], X1S[rs, :], writes=[("xa", b6)])
                p.dma("sp", ya[b3][:], YS[rs, :], writes=[("ya", b3)])
                p.dma("sp", yb_[b3][:], YS[NOWN + tt * 128:NOWN + (tt + 1) * 128, :], writes=[("yb", b3)])
                p.op("pool", lambda e, b3=b3: e.tensor_tensor(ya[b3][:], ya[b3][:], yb_[b3][:], ALU.add), reads=[("ya", b3), ("yb", b3)], writes=[("ya", b3)])
                p.op("dve", lambda e, b3=b3, b6=b6: e.tensor_tensor(xa[b6][:], xa[b6][:], ya[b3][:], ALU.add), reads=[("xa", b6), ("ya", b3)], writes=[("xa", b6)])
                p.op("act", lambda e, b6=b6, tt=tt: e.activation(junk[:], xa[b6][:], AF.Square, accum_out=st3[:, tt:tt + 1]), reads=[("xa", b6), "st3z"], writes=["junkD", ("ss3", tt)])
            gs = slice(4 * g4, 4 * g4 + 4)
            st3g = st3[:, gs]
            p.op("dve", lambda e, st3g=st3g: e.tensor_scalar(st3g, st3g, 1.0 / D, EPS, ALU.mult, ALU.add), reads=[("ss3", 4 * g4 + i) for i in range(4)], writes=[("st3", g4)])
            p.op("act", lambda e, st3g=st3g: e.activation(st3g, st3g, AF.Ln), reads=[("st3", g4)], writes=[("st3", g4)])
            p.op("act", lambda e, st3g=st3g: e.activation(st3g, st3g, AF.Exp, scale=-0.5), reads=[("st3", g4)], writes=[("st3", g4)])
            for i in range(4):
                tt = 4 * g4 + i
                b3 = tt % 3
                b6 = tt % 6
                p.op("dve", lambda e, b3=b3, b6=b6, tt=tt: e.scalar_tensor_tensor(outt[b3][:], xa[b6][:], st3[:, tt:tt + 1], gfbc[:], ALU.mult, ALU.mult),
                     reads=[("xa", b6), ("st3", g4), "gfbc"], writes=[("outt", b3)])
                finals.append(p.dma("sp", y_out[tt * 128:(tt + 1) * 128, :], outt[b3][:], reads=[("outt", b3)]))

    phase_D()

    p.finals = finals
    p.finalize()
    p.run_block()
    info = dict(n_ops=len(p.ops), max_sem=p.max_sem, sbuf_peak=sb.peak - sb.base)
    return nc, info


_CACHE = {}


def _consts():
    identb = np.eye(128, dtype=np.float32).astype(ml_dtypes.bfloat16)
    identf = np.eye(128, dtype=np.float32)
    s = np.arange(128)[:, None]
    t = np.arange(128)[None, :]
    tri = (s <= t).astype(np.float32)
    mneg = np.where(s <= t, 0.0, NEGM).astype(np.float32).astype(ml_dtypes.bfloat16)
    tris = (s < t).astype(np.float32)
    return identb, identf, tri, mneg, tris


def run_layer(inputs, NPREV, NOWN, debug=False):
    x = np.asarray(inputs["x"], dtype=np.float32)
    B, T, _ = x.shape
    nhalf = T // NOWN
    ncores = B * nhalf
    key = (NPREV, NOWN, debug)
    if key not in _CACHE:
        _CACHE[key] = build_program(NPREV, NOWN, debug=debug)
    nc, info = _CACHE[key]
    f = lambda k: np.ascontiguousarray(np.asarray(inputs[k], dtype=np.float32)[0])
    identb, identf, tri, mneg, tris = _consts()
    btab, bmsk, bneg = _bias_tables(np.asarray(inputs["rel_bias"], dtype=np.float32))
    shared = {
        "w_in": f("w_in"), "b_forget": f("b_forget"), "attn_norm": f("attn_norm"),
        "out_norm_dil": f("out_norm_dil"), "out_norm_fox": f("out_norm_fox"), "w_out": f("w_out"),
        "ffn_norm": f("ffn_norm"), "w_rg": f("w_router_group"), "b_rg": f("b_router_group"),
        "w_re": f("w_router_expert"), "b_re": f("b_router_expert"), "w_eg": f("w_expert_gate"),
        "w_eu": f("w_expert_up"), "w_ed": f("w_expert_down"),
        "final_norm": np.ascontiguousarray(np.asarray(inputs["final_norm"], dtype=np.float32)),
        "btab": btab, "bmsk": bmsk, "bneg": bneg, "identb": identb, "identf": identf, "tri": tri,
        "mneg": mneg, "tris": tris,
    }
    NTO_ = NOWN // 128
    NTL_ = 2 * NOWN // 128 + 32
    shared["tokid"] = (np.arange(NTO_)[None, :] * 128 + np.arange(128)[:, None]).astype(np.float32)
    shared["jv"] = np.broadcast_to((np.arange(NTL_) * 128.0)[None, :], (128, NTL_)).astype(np.float32).copy()
    shared["pidx"] = np.arange(128, dtype=np.float32).reshape(128, 1)
    si = np.zeros((NTL_ * 128, 4), np.int32)
    si[:, 1] = 2 * NOWN + np.arange(NTL_ * 128)
    shared["slot_init"] = si
    in_maps = []
    for core in range(ncores):
        b, hidx = divmod(core, nhalf)
        own0 = hidx * NOWN
        xs = np.zeros((NPREV + NOWN, D), np.float32)
        npv = min(NPREV, own0)
        if npv > 0:
            xs[NPREV - npv:NPREV] = x[b, own0 - npv:own0]
        xs[NPREV:] = x[b, own0:own0 + NOWN]
        pv = 1.0 if npv == NPREV else 0.0
        assert npv in (0, NPREV)
        m = dict(shared)
        m["xs"] = xs
        m["pvt"] = np.full((128, 64), pv, np.float32)
        in_maps.append(m)
    res = run_bass_kernel_spmd(nc, in_maps, core_ids=list(range(ncores)))
    out = np.zeros((B, T, D), np.float32)
    for core in range(ncores):
        b, hidx = divmod(core, nhalf)
        out[b, hidx * NOWN:(hidx + 1) * NOWN] = res.results[core]["y"]
    return out, res, info


def kernel(**inputs):
    out, _, _ = run_layer(inputs, 4096, 4096)
    return out
```

```python
import math
import numpy as np
import ml_dtypes
import concourse.bass as bass
import concourse.mybir as mybir
from concourse.bass_utils import run_bass_kernel_spmd

F32 = mybir.dt.float32
BF16 = mybir.dt.bfloat16
I32 = mybir.dt.int32
AF = mybir.ActivationFunctionType
ALU = mybir.AluOpType
AX = mybir.AxisListType

ENGS = ("pe", "act", "dve", "pool", "sp")
EPS = 1e-6
NEGM = -30000.0
D = 1024
KC = 8
DIN = 3080
NEXP = 32
DEXP = 256
import os as _os
PIPE_B = _os.environ.get('PIPE_B', '1') == '1'
PIPE_C = _os.environ.get('PIPE_C', '1') == '1'
SKIP_D3 = _os.environ.get('SKIP_D3', '0') == '1'
NO_WALL = _os.environ.get('NO_WALL', '0') == '1'
SKIP_D1B = _os.environ.get('SKIP_D1B', '0') == '1'
SKIP_D2 = _os.environ.get('SKIP_D2', '0') == '1'
D2_STOP = int(_os.environ.get('D2_STOP', '0'))


class Prog:
    def __init__(self, nc, n_dma_slots=12):
        self.nc = nc
        self.ops = []
        self.eng_ops = {e: [] for e in ENGS}
        self.last_writer = {}
        self.readers = {}
        self.n_dma_slots = n_dma_slots
        self.dma_rr = {e: 0 for e in ENGS}
        self.slot_last = {}
        self.finals = []
        self.last_op = {}

    def _add(self, domain, eng, fn, reads, writes, is_dma):
        idx = len(self.ops)
        deps = set()
        raw = set()
        for k in reads:
            w = self.last_writer.get(k)
            if w is not None:
                deps.add(w)
                raw.add(w)
        for k in writes:
            w = self.last_writer.get(k)
            if w is not None:
                deps.add(w)
            for r in self.readers.get(k, {}).values():
                deps.add(r)
        for k in writes:
            self.last_writer[k] = idx
            self.readers[k] = {}
        for k in reads:
            self.readers.setdefault(k, {})[domain] = idx
        if is_dma:
            prev = self.slot_last.get(domain)
            if prev is not None:
                deps.add(prev)
            self.slot_last[domain] = idx
        deps.discard(idx)
        raw.discard(idx)
        self.ops.append([domain, eng, fn, sorted(deps), is_dma, raw])
        self.eng_ops[eng].append(idx)
        self.last_op[domain] = idx
        return idx

    def op(self, eng, fn, reads=(), writes=()):
        return self._add(eng, eng, fn, reads, writes, False)

    def dma(self, eng, out, in_, reads=(), writes=(), **kw):
        slot = self.dma_rr[eng]
        self.dma_rr[eng] = (slot + 1) % self.n_dma_slots
        domain = ("dma", eng, slot)
        fn = lambda e, out=out, in_=in_, kw=kw: e.dma_start(out=out, in_=in_, **kw)
        return self._add(domain, eng, fn, reads, writes, True)

    def dma_custom(self, eng, fn, reads=(), writes=()):
        slot = self.dma_rr[eng]
        self.dma_rr[eng] = (slot + 1) % self.n_dma_slots
        domain = ("dma", eng, slot)
        return self._add(domain, eng, fn, reads, writes, True)

    def barrier(self):
        lasts = dict(self.last_op)
        for eng in ENGS:
            idx = len(self.ops)
            deps = sorted(v for d, v in lasts.items() if d != eng)
            self.ops.append([eng, eng, None, deps, False, set()])
            self.eng_ops[eng].append(idx)
            self.last_op[eng] = idx

    def finalize(self):
        nc = self.nc
        ops = self.ops
        need = [False] * len(ops)
        waits = [None] * len(ops)
        for d in self.finals:
            need[d] = True
        for idx, o in enumerate(ops):
            if o[4]:
                need[idx] = True
        seen = {e: {} for e in ENGS}
        for idx, (domain, eng, fn, deps, is_dma, raw) in enumerate(ops):
            wl = []
            best = {}
            for d in deps:
                dd = ops[d][0]
                if dd == eng and not is_dma and not ops[d][4]:
                    if eng == "pe" or d not in raw:
                        continue
                if dd not in best or best[dd] < d:
                    best[dd] = d
            for dd, d in best.items():
                if seen[eng].get(dd, -1) >= d:
                    continue
                seen[eng][dd] = d
                wl.append(d)
                need[d] = True
            waits[idx] = wl
        self.sems = {}
        cnt = {}
        token = [None] * len(ops)
        for idx, (domain, eng, fn, deps, is_dma, raw) in enumerate(ops):
            if domain not in self.sems:
                name = "s_" + ("_".join(str(x) for x in domain) if isinstance(domain, tuple) else domain)
                self.sems[domain] = nc.alloc_semaphore(name)
                cnt[domain] = 0
            if need[idx]:
                cnt[domain] += 16 if is_dma else 1
                token[idx] = (self.sems[domain], cnt[domain])
        self.max_sem = max(cnt.values()) if cnt else 0
        self.token = token
        self.waits = waits
        return self

    def emit_engine(self, eng, e):
        ops = self.ops
        for idx in self.eng_ops[eng]:
            domain, _, fn, deps, is_dma, raw = ops[idx]
            for d in self.waits[idx]:
                sem, val = self.token[d]
                e.wait_ge(sem, val)
            if fn is None:
                if self.token[idx] is None:
                    continue
                ins = e.nop()
            else:
                ins = fn(e)
            if self.token[idx] is not None:
                sem, val = self.token[idx]
                ins.then_inc(sem, 16 if is_dma else 1)

    def run_block(self):
        nc = self.nc
        with nc.Block() as block:
            @block.sync
            def _(e):
                self.emit_engine("sp", e)
                for d in self.finals:
                    sem, val = self.token[d]
                    e.wait_ge(sem, val)

            @block.tensor
            def _(e):
                self.emit_engine("pe", e)

            @block.scalar
            def _(e):
                self.emit_engine("act", e)

            @block.vector
            def _(e):
                self.emit_engine("dve", e)

            @block.gpsimd
            def _(e):
                self.emit_engine("pool", e)


class SBAlloc:
    def __init__(self, nc):
        self.nc = nc
        self.base = (nc.sbuf_base + 63) // 64 * 64
        self.top = nc.sbuf_top
        self.off = self.base
        self.n = 0
        self.peak = 0

    def alloc(self, name, shape, dt):
        esz = 4 if dt in (F32, I32) else 2
        nbytes = int(np.prod(shape[1:])) * esz
        nbytes = (nbytes + 63) // 64 * 64
        self.n += 1
        t = self.nc.alloc_sbuf_tensor_at(f"{name}_{self.n}", list(shape), dt, offset=self.off)
        self.off += nbytes
        self.peak = max(self.peak, self.off)
        assert self.off <= self.top, f"SBUF overflow allocating {name}: {self.off} > {self.top}"
        return t

    def mark(self):
        return self.off

    def reset(self, m):
        self.off = m


def _t5_bucket_np(dist):
    max_exact = 16
    d32 = np.maximum(dist, 1).astype(np.float32)
    large = max_exact + (np.log(d32 / np.float32(max_exact)) / np.float32(math.log(2048 / max_exact))
                         * np.float32(32 - max_exact)).astype(np.int32)
    large = np.minimum(large, 31)
    return np.where(dist < max_exact, dist, large)


def _bias_tables(rel_bias):
    k = np.arange(128)[:, None]
    c = np.arange(256)[None, :]
    iq = np.where(c < 128, c, c - 128)
    m = np.where(c < 128, iq - k, iq - k + 128)
    valid = (m >= 0) & (m <= 128)
    mc = np.clip(m, 0, 128)
    btab = np.zeros((128, 24, 256), np.float32)
    for di, dil in enumerate((1, 4, 16)):
        bidx = _t5_bucket_np(mc * dil)
        for h in range(8):
            btab[:, di * 8 + h, :] = rel_bias[bidx, h]
    bmsk = valid.astype(np.float32)
    bneg = np.where(valid, 0.0, NEGM).astype(np.float32)
    return btab.reshape(128, 24 * 256), bmsk, bneg


def build_program(NPREV, NOWN, debug=False):
    assert NPREV % 2048 == 0 and NPREV >= 2048 and NOWN % 2048 == 0
    NTOK = NPREV + NOWN
    NT = NTOK // 128
    NTP = NPREV // 128
    NTO = NOWN // 128
    NG = NTOK // 512
    NCH = NOWN // 2048
    NQC = NOWN // 1024
    KD0 = NPREV - 2048
    NKD = NOWN + 2048

    nc = bass.Bass("TRN2", target_bir_lowering=False)
    NTL_ = 2 * NOWN // 128 + 32

    def din(name, shape, dt=F32):
        return nc.dram_tensor(name, list(shape), dt, kind="ExternalInput").ap()

    xs = din("xs", [NTOK, D])
    pvt = din("pvt", [128, 64])
    w_in = din("w_in", [D, DIN])
    b_forget = din("b_forget", [8])
    attn_norm = din("attn_norm", [D])
    out_norm_dil = din("out_norm_dil", [512])
    out_norm_fox = din("out_norm_fox", [512])
    w_out = din("w_out", [D, D])
    ffn_norm = din("ffn_norm", [D])
    w_rg = din("w_rg", [D, 4])
    b_rg = din("b_rg", [4])
    w_re = din("w_re", [D, 32])
    b_re = din("b_re", [32])
    w_eg = din("w_eg", [NEXP, D, DEXP])
    w_eu = din("w_eu", [NEXP, D, DEXP])
    w_ed = din("w_ed", [NEXP, DEXP, D])
    final_norm = din("final_norm", [D])
    btab_d = din("btab", [128, 24 * 256])
    bmsk_d = din("bmsk", [128, 256])
    bneg_d = din("bneg", [128, 256])
    identb_d = din("identb", [128, 128], BF16)
    identf_d = din("identf", [128, 128])
    tri_d = din("tri", [128, 128])
    mneg_d = din("mneg", [128, 128], BF16)
    NTL_ = 2 * NOWN // 128 + 32
    tris_d = din("tris", [128, 128])
    tokid_d = din("tokid", [128, NTO])
    jv_d = din("jv", [128, NTL_])
    pidx_d = din("pidx", [128, 1])
    slot_init_d = din("slot_init", [NTL_ * 128, 4], I32)
    y_out = nc.dram_tensor("y", [NOWN, D], F32, kind="ExternalOutput").ap()

    def dscr(name, shape, dt):
        return nc.dram_tensor(name, list(shape), dt).ap()

    QdT = dscr("QdT", [512, NOWN], BF16)
    KdT = dscr("KdT", [512, NKD], BF16)
    VdS = dscr("VdS", [NKD, 512], BF16)
    QfT = dscr("QfT", [512, NOWN], BF16)
    QfA = dscr("QfA", [8, 3, NOWN], BF16)
    KfT = dscr("KfT", [512, NTOK], BF16)
    VfS = dscr("VfS", [NTOK, 512], BF16)
    cS = dscr("cS", [NTOK, 8], F32)
    OcS = dscr("OcS", [1024, NOWN], BF16)
    WALL = dscr("WALL", [NEXP * 128, 6144], BF16)
    H2S = dscr("H2S", [NOWN, D], BF16)
    X1S = dscr("X1S", [NOWN, D], F32)
    YS = dscr("YS", [2 * NOWN + NTL_ * 128, D], F32)
    SLOTINFO = dscr("SLOTINFO", [NTL_ * 128, 4], I32)

    dbg = {}
    if debug:
        dbg["d_ctok"] = nc.dram_tensor("d_ctok", [128, NT * 8], F32, kind="ExternalOutput").ap()
        dbg["d_oc"] = nc.dram_tensor("d_oc", [1024, NOWN], F32, kind="ExternalOutput").ap()
        dbg["d_x1"] = nc.dram_tensor("d_x1", [NOWN, D], F32, kind="ExternalOutput").ap()
        dbg["d_comb"] = nc.dram_tensor("d_comb", [NOWN, 32], F32, kind="ExternalOutput").ap()
        dbg["d_slot"] = nc.dram_tensor("d_slot", [128, NTO * 2], F32, kind="ExternalOutput").ap()
        dbg["d_eid"] = nc.dram_tensor("d_eid", [128, NTL_], F32, kind="ExternalOutput").ap()
        dbg["d_cnt"] = nc.dram_tensor("d_cnt", [128, 32 * 3], F32, kind="ExternalOutput").ap()

    sb = SBAlloc(nc)
    ps = nc.alloc_psum_tensor("ps", [128, 8, 512], F32)
    p = Prog(nc)
    finals = []

    def psb(b, n=1):
        if n == 1:
            return ps[:, b, :]
        return ps[:, b:b + n, :].rearrange("p b c -> p (b c)")

    identb = sb.alloc("identb", [128, 128], BF16)
    identf = sb.alloc("identf", [128, 128], F32)
    mneg = sb.alloc("mneg", [128, 128], BF16)
    pvb = sb.alloc("pvb", [128, 64], BF16)
    pvf = sb.alloc("pvf", [128, 64], F32)
    ones_bf = sb.alloc("ones_bf", [128, 1], BF16)
    ctok = sb.alloc("ctok", [128, NT, 8], F32)
    crefbc = sb.alloc("crefbc", [128, NQC, 8], F32)
    p.dma("sp", identb[:], identb_d[:, :], writes=["identb"])
    p.dma("sp", identf[:], identf_d[:, :], writes=["identf"])
    p.dma("sp", mneg[:], mneg_d[:, :], writes=["mneg"])
    p.dma("sp", pvf[:], pvt[:, :], writes=["pvf"])
    p.op("dve", lambda e: e.tensor_copy(pvb[:], pvf[:]), reads=["pvf"], writes=["pvb"])
    p.op("dve", lambda e: e.memset(ones_bf[:], 1.0), writes=["ones_bf"])
    persist_mark = sb.mark()

    def phase_A():
        win = sb.alloc("win", [128, KC, DIN], BF16)
        gbc = sb.alloc("gbc", [128, D], F32)
        bfb = sb.alloc("bfb", [128, 8], F32)
        NXB, NHB = 4, 4
        xt = [sb.alloc(f"xt{i}", [128, D], F32) for i in range(NXB)]
        junk = sb.alloc("junk", [128, D], BF16)
        hb = [sb.alloc(f"hb{i}", [128, D], BF16) for i in range(NHB)]
        hT = [sb.alloc(f"hT{i}", [128, KC, 512], BF16) for i in range(2)]
        qst = [sb.alloc(f"qst{i}", [128, 512], BF16) for i in range(4)]
        vst = [sb.alloc(f"vst{i}", [128, 512], BF16) for i in range(4)]
        ssA = sb.alloc("ssA", [128, NT], F32)
        rsA = sb.alloc("rsA", [128, NT], F32)
        fall = sb.alloc("fall", [128, NT, 8], F32)
        tri = sb.alloc("tri", [128, 128], F32)
        onesf = sb.alloc("onesf", [128, 128], F32)

        for c in range(KC):
            p.dma("pool", win[:, c, :], w_in[c * 128:(c + 1) * 128, :], writes=[("win", c)])
        p.dma("sp", gbc[:], attn_norm.partition_broadcast(128), writes=["gbc"])
        p.dma("sp", bfb[:], b_forget.partition_broadcast(128), writes=["bfb"])
        p.dma("sp", tri[:], tri_d[:, :], writes=["tri"])
        p.op("dve", lambda e: e.memset(ssA[:], 0.0), writes=["ssA"])
        p.op("dve", lambda e: e.memset(onesf[:], 1.0), writes=["onesf"])

        pT = [ps[:, 0, :].bitcast(BF16), ps[:, 1, :].bitcast(BF16)]
        cnt_qk = [0]
        cnt_v = [0]
        cnt_ev = [0]
        winkeys = [("win", c) for c in range(KC)]

        for g in range(NG):
            tok0 = g * 512
            own = tok0 >= NPREV
            need_kd = own or tok0 >= KD0
            for tt in range(4):
                ti = 4 * g + tt
                xb = xt[ti % NXB]
                p.dma("sp", xb[:], xs[ti * 128:(ti + 1) * 128, :], writes=[("xt", ti % NXB)])
                p.op("act", lambda e, xb=xb, ti=ti: e.activation(junk[:], xb[:], AF.Square, accum_out=ssA[:, ti:ti + 1]),
                     reads=[("xt", ti % NXB), "ssA"], writes=["junk", ("ss", ti)])
            g4 = slice(4 * g, 4 * g + 4)
            p.op("dve", lambda e, g4=g4: e.tensor_scalar(rsA[:, g4], ssA[:, g4], 1.0 / D, EPS, ALU.mult, ALU.add),
                 reads=[("ss", 4 * g + i) for i in range(4)], writes=[("rs", g)])
            p.op("act", lambda e, g4=g4: e.activation(rsA[:, g4], rsA[:, g4], AF.Ln), reads=[("rs", g)], writes=[("rs", g)])
            p.op("act", lambda e, g4=g4: e.activation(rsA[:, g4], rsA[:, g4], AF.Exp, scale=-0.5), reads=[("rs", g)], writes=[("rs", g)])
            for tt in range(4):
                ti = 4 * g + tt
                xb = xt[ti % NXB]
                hbt = hb[ti % NHB]
                p.op("dve", lambda e, xb=xb, hbt=hbt, ti=ti: e.scalar_tensor_tensor(hbt[:], xb[:], rsA[:, ti:ti + 1], gbc[:], ALU.mult, ALU.mult),
                     reads=[("xt", ti % NXB), ("rs", g), "gbc"], writes=[("hb", ti % NHB)])
                pt = pT[ti % 2]
                for c in range(KC):
                    p.op("pe", lambda e, pt=pt, hbt=hbt, c=c: e.transpose(pt[:, c * 128:(c + 1) * 128], hbt[:, c * 128:(c + 1) * 128], identb[:]),
                         reads=[("hb", ti % NHB), "identb"], writes=[("pT", ti % 2)])
                dst = hT[g % 2][:, :, tt * 128:(tt + 1) * 128]
                src = pt.rearrange("p (c t) -> p c t", c=KC)
                if tt % 2 == 0:
                    p.op("act", lambda e, dst=dst, src=src: e.copy(dst, src), reads=[("pT", ti % 2)], writes=[("hT", g % 2, tt)])
                else:
                    p.op("dve", lambda e, dst=dst, src=src: e.tensor_copy(dst, src), reads=[("pT", ti % 2)], writes=[("hT", g % 2, tt)])
            hTk = [("hT", g % 2, tt) for tt in range(4)]
            blocks = []
            if own:
                o0 = tok0 - NPREV
                for b in range(4):
                    blocks.append((0 + b * 128, 0.125, QdT[b * 128:(b + 1) * 128, o0:o0 + 512]))
                for b in range(4):
                    blocks.append((1536 + b * 128, 0.125, QfT[b * 128:(b + 1) * 128, o0:o0 + 512]))
            if need_kd:
                k0 = tok0 - KD0
                for b in range(4):
                    blocks.append((512 + b * 128, 1.0, KdT[b * 128:(b + 1) * 128, k0:k0 + 512]))
            for b in range(4):
                blocks.append((2048 + b * 128, 1.0, KfT[b * 128:(b + 1) * 128, tok0:tok0 + 512]))
            for (col, scale, dst) in blocks:
                i = cnt_qk[0]
                cnt_qk[0] += 1
                pq = ps[:, 2 + i % 3, :]
                for c in range(KC):
                    p.op("pe", lambda e, pq=pq, c=c, col=col, g=g: e.matmul(pq, win[:, c, col:col + 128], hT[g % 2][:, c, :], start=(c == 0), stop=(c == KC - 1)),
                         reads=winkeys + hTk, writes=[("pQK", i % 3)])
                st = qst[i % 4]
                j = cnt_ev[0]
                cnt_ev[0] += 1
                if j % 2 == 0:
                    p.op("act", lambda e, st=st, pq=pq, scale=scale: e.activation(st[:], pq, AF.Copy, scale=scale),
                         reads=[("pQK", i % 3)], writes=[("qst", i % 4)])
                else:
                    p.op("dve", lambda e, st=st, pq=pq, scale=scale: e.tensor_scalar(st[:], pq, scale, None, ALU.mult),
                         reads=[("pQK", i % 3)], writes=[("qst", i % 4)])
                p.dma("pool", dst, st[:], reads=[("qst", i % 4)])
            for tt in range(4):
                ti = 4 * g + tt
                vlist = []
                if need_kd:
                    k0 = tok0 - KD0 + tt * 128
                    vlist.append((1024, VdS[k0:k0 + 128, :]))
                vlist.append((2560, VfS[ti * 128:(ti + 1) * 128, :]))
                for (col, dst) in vlist:
                    i = cnt_v[0]
                    cnt_v[0] += 1
                    pv = ps[:, 5 + i % 2, :]
                    for c in range(KC):
                        p.op("pe", lambda e, pv=pv, c=c, col=col, g=g, tt=tt: e.matmul(pv, hT[g % 2][:, c, tt * 128:(tt + 1) * 128], win[:, c, col:col + 512], start=(c == 0), stop=(c == KC - 1)),
                             reads=winkeys + [("hT", g % 2, tt)], writes=[("pV", i % 2)])
                    st = vst[i % 4]
                    j = cnt_ev[0]
                    cnt_ev[0] += 1
                    if j % 2 == 0:
                        p.op("act", lambda e, st=st, pv=pv: e.copy(st[:], pv), reads=[("pV", i % 2)], writes=[("vst", i % 4)])
                    else:
                        p.op("dve", lambda e, st=st, pv=pv: e.tensor_copy(st[:], pv), reads=[("pV", i % 2)], writes=[("vst", i % 4)])
                    p.dma("pool", dst, st[:], reads=[("vst", i % 4)])
                pf = ps[:, 7, (ti % 8) * 8:(ti % 8) * 8 + 8]
                for c in range(KC):
                    p.op("pe", lambda e, pf=pf, c=c, g=g, tt=tt: e.matmul(pf, hT[g % 2][:, c, tt * 128:(tt + 1) * 128], win[:, c, 3072:3080], start=(c == 0), stop=(c == KC - 1)),
                         reads=winkeys + [("hT", g % 2, tt)], writes=["pF7"])
                p.op("dve", lambda e, pf=pf, ti=ti: e.tensor_tensor(fall[:, ti, :], pf, bfb[:], ALU.add),
                     reads=["pF7", "bfb"], writes=["fall"])

        NF = NT * 8
        lall = sb.alloc("lall", [128, NF], F32)
        wcs = sb.alloc("wcs", [128, NF], F32)
        tA = sb.alloc("tA", [128, NF], F32)
        tB = sb.alloc("tB", [128, NF], F32)
        fflat = fall[:].rearrange("p t h -> p (t h)")
        p.op("act", lambda e: e.activation(lall[:], fflat, AF.Exp, scale=-1.0), reads=["fall"], writes=["lall"])
        p.op("act", lambda e: e.activation(lall[:], lall[:], AF.Ln, bias=1.0), reads=["lall"], writes=["lall"])
        nbk = (NF + 511) // 512
        assert nbk <= 2
        for bk in range(nbk):
            cs = slice(bk * 512, min(NF, (bk + 1) * 512))
            w = cs.stop - cs.start
            p.op("pe", lambda e, cs=cs, w=w, bk=bk: e.matmul(ps[:, 2 + bk, 0:w], tri[:], lall[:, cs], start=True, stop=True),
                 reads=["tri", "lall"], writes=[("pQK", bk)])
            p.op("pe", lambda e, cs=cs, w=w, bk=bk: e.matmul(ps[:, 5 + bk, 0:w], onesf[:], lall[:, cs], start=True, stop=True),
                 reads=["onesf", "lall"], writes=[("pV", bk)])
            p.op("dve", lambda e, cs=cs, w=w, bk=bk: e.tensor_copy(wcs[:, cs], ps[:, 2 + bk, 0:w]), reads=[("pQK", bk)], writes=["wcs"])
            p.op("act", lambda e, cs=cs, w=w, bk=bk: e.copy(tA[:, cs], ps[:, 5 + bk, 0:w]), reads=[("pV", bk)], writes=["tA"])
        src, dst = tA, tB
        k = 1
        while k < NT:
            kk = k * 8
            p.op("dve", lambda e, src=src, dst=dst, kk=kk: e.tensor_copy(dst[:, 0:kk], src[:, 0:kk]), reads=["tA", "tB"], writes=["tA", "tB"])
            p.op("dve", lambda e, src=src, dst=dst, kk=kk: e.tensor_tensor(dst[:, kk:NF], src[:, kk:NF], src[:, 0:NF - kk], ALU.add), reads=["tA", "tB"], writes=["tA", "tB"])
            src, dst = dst, src
            k *= 2
        incl = src
        tot = sb.alloc("tot", [128, NF], F32)
        for bk in range(nbk):
            cs = slice(bk * 512, min(NF, (bk + 1) * 512))
            w = cs.stop - cs.start
            p.op("act", lambda e, cs=cs, w=w, bk=bk: e.copy(tot[:, cs], ps[:, 5 + bk, 0:w]), reads=[("pV", bk)], writes=["tot"])
        cflat = ctok[:].rearrange("p t h -> p (t h)")
        p.op("dve", lambda e: e.tensor_tensor(wcs[:], wcs[:], incl[:], ALU.add), reads=["wcs", "tA", "tB"], writes=["wcs"])
        p.op("dve", lambda e: e.tensor_tensor(wcs[:], wcs[:], tot[:], ALU.subtract), reads=["wcs", "tot"], writes=["wcs"])
        p.op("dve", lambda e: e.tensor_scalar(cflat, wcs[:], -1.0, None, ALU.mult), reads=["wcs"], writes=["ctok"])
        p.dma("sp", cS.rearrange("(t p) h -> p t h", p=128), ctok[:], reads=["ctok"], writes=["cS"])
        if debug:
            finals.append(p.dma("sp", dbg["d_ctok"][:, :], cflat, reads=["ctok"]))
        p.dma("sp", crefbc[:], bass.AP(cS.tensor, NPREV * 8, [[0, 128], [1024 * 8, NQC], [1, 8]]),
              reads=["cS"], writes=["crefbc"])
        cT = sb.alloc("cT", [8, NOWN], F32)
        r1 = sb.alloc("r1", [8, NOWN], F32)
        aug = [sb.alloc(f"aug{i}", [8, NOWN], BF16) for i in range(3)]
        for t4 in range(NTO // 4):
            for i in range(4):
                ti = NTP + 4 * t4 + i
                p.op("pe", lambda e, ti=ti, i=i: e.transpose(ps[0:8, 2, i * 128:(i + 1) * 128], ctok[:, ti, :], identf[:]),
                     reads=["ctok", "identf"], writes=[("pQK", 0)])
            p.op("dve", lambda e, t4=t4: e.tensor_copy(cT[:, t4 * 512:(t4 + 1) * 512], ps[0:8, 2, :]), reads=[("pQK", 0)], writes=["cT"])
        for qc in range(NQC):
            qs = slice(qc * 1024, (qc + 1) * 1024)
            p.op("dve", lambda e, qs=qs: e.tensor_scalar(r1[:, qs], cT[:, qs], cT[:, qs.start:qs.start + 1], None, ALU.subtract),
                 reads=["cT"], writes=["r1"])
        for i in range(3):
            p.op("dve", lambda e, i=i: e.tensor_copy(aug[i][:], r1[:]), reads=["r1"], writes=[("aug", i)])
            if i < 2:
                p.op("dve", lambda e, i=i: e.tensor_tensor(r1[:], r1[:], aug[i][:], ALU.subtract), reads=["r1", ("aug", i)], writes=["r1"])
            p.dma("sp", QfA[:, i, :], aug[i][:], reads=[("aug", i)])

    phase_A()
    p.barrier()
    sb.reset(persist_mark)

    def phase_B():
        BM = sb.alloc("BM", [128, 24, 256], BF16)
        bt = sb.alloc("bt", [128, 24, 256], F32)
        msk = sb.alloc("msk", [128, 256], F32)
        neg = sb.alloc("neg", [128, 256], F32)
        p.dma("sp", bt[:].rearrange("p a b -> p (a b)"), btab_d[:, :], writes=["bt"])
        p.dma("sp", msk[:], bmsk_d[:, :], writes=["msk"])
        p.dma("sp", neg[:], bneg_d[:, :], writes=["neg"])
        for i in range(24):
            p.op("pool", lambda e, i=i: e.tensor_tensor(bt[:, i, :], bt[:, i, :], msk[:], ALU.mult), reads=["bt", "msk"], writes=["bt"])
            p.op("pool", lambda e, i=i: e.tensor_tensor(BM[:, i, :], bt[:, i, :], neg[:], ALU.add), reads=["bt", "neg"], writes=["BM"])
        kT = [sb.alloc(f"kT{i}", [128, 4096], BF16) for i in range(2)]
        qT = [sb.alloc(f"qT{i}", [128, 2048], BF16) for i in range(2)]
        q4 = [sb.alloc(f"q4{i}", [128, 2048], BF16) for i in range(2)]
        q16 = [sb.alloc(f"q16{i}", [128, 2048], BF16) for i in range(2)]
        vA = [[sb.alloc(f"vA{i}{b}", [128, 32, 128], BF16) for b in range(3)] for i in range(2)]
        Pb = [sb.alloc(f"Pb{i}", [128, 512], BF16) for i in range(4)]
        acc = sb.alloc("acc", [128, 2048], F32)
        rd0 = sb.alloc("rd0", [128, 2048], F32)
        onb = [sb.alloc(f"onb{i}", [128, 2048], BF16) for i in range(2)]
        Oflat = psb(0, 4)
        scnt = [0]
        pcnt = [0]

        steps = []
        for ch in range(NCH):
            for hp in range(4):
                for hh in range(2):
                    for br, dil in enumerate((1, 4, 16)):
                        kb_ = (ch * 4 + hp) % 2
                        qt, q4t, q16t = qT[kb_], q4[kb_], q16[kb_]
                        blk = []
                        if dil == 1:
                            for kb in range(-1, 16):
                                kc = slice(2048 + 128 * kb, 2048 + 128 * kb + 128)
                                if kb == -1:
                                    blk.append((kc, qt, slice(0, 128), 128, slice(128, 256), 16 + kb, slice(0, 128)))
                                elif kb == 15:
                                    blk.append((kc, qt, slice(1920, 2048), 128, slice(0, 128), 16 + kb, slice(1920, 2048)))
                                else:
                                    blk.append((kc, qt, slice(128 * kb, 128 * kb + 256), 256, slice(0, 256), 16 + kb, slice(128 * kb, 128 * kb + 256)))
                        elif dil == 4:
                            for r in range(4):
                                for g in range(-1, 4):
                                    kc = slice(2048 + 512 * g + r, 2048 + 512 * g + r + 509, 4)
                                    n = 4 * (g + 4) + r
                                    if g == -1:
                                        blk.append((kc, q4t, slice(r * 512, r * 512 + 128), 128, slice(128, 256), n, slice(r * 512, r * 512 + 128)))
                                    elif g == 3:
                                        blk.append((kc, q4t, slice(r * 512 + 384, r * 512 + 512), 128, slice(0, 128), n, slice(r * 512 + 384, r * 512 + 512)))
                                    else:
                                        blk.append((kc, q4t, slice(r * 512 + 128 * g, r * 512 + 128 * g + 256), 256, slice(0, 256), n, slice(r * 512 + 128 * g, r * 512 + 128 * g + 256)))
                        else:
                            for r in range(16):
                                for G in (-1, 0):
                                    kc = slice(2048 + 2048 * G + r, 2048 + 2048 * G + r + 2033, 16)
                                    n = 16 * (G + 1) + r
                                    blk.append((kc, q16t, slice(r * 128, r * 128 + 128), 128, slice(128, 256) if G == -1 else slice(0, 128), n, slice(r * 128, r * 128 + 128)))
                        npairs = (len(blk) + 1) // 2
                        for pi_ in range(npairs):
                            steps.append(dict(ch=ch, hp=hp, hh=hh, br=br, dil=dil, pair=blk[2 * pi_:2 * pi_ + 2],
                                              first_br=(pi_ == 0), last_br=(pi_ == npairs - 1)))
        for i, st in enumerate(steps):
            st["i"] = i
        touched = set()

        def pre_qk(st):
            ch, hp, hh, br = st["ch"], st["hp"], st["hh"], st["br"]
            if not (st["first_br"] and br == 0):
                return
            kb_ = (ch * 4 + hp) % 2
            if hh == 0:
                if hp == 0 and ch == 0:
                    for vb in range(2):
                        for b3 in range(3):
                            if ch == 0:
                                p.op("pool", lambda e, vb=vb, b3=b3: e.tensor_copy(vA[vb][b3][:, 0:16, 64:128], pvb[:].unsqueeze(1).broadcast_to([128, 16, 64])),
                                     reads=["pvb"], writes=[("vAc", vb, b3)])
                                p.op("pool", lambda e, vb=vb, b3=b3: e.memset(vA[vb][b3][:, 16:32, 64:128], 1.0), writes=[("vAc", vb, b3)])
                kt, qt, q4t, q16t = kT[kb_], qT[kb_], q4[kb_], q16[kb_]
                p.dma("sp", kt[:], KdT[hp * 128:(hp + 1) * 128, ch * 2048:ch * 2048 + 4096], writes=[("kT", kb_)])
                p.dma("sp", qt[:], QdT[hp * 128:(hp + 1) * 128, ch * 2048:(ch + 1) * 2048], writes=[("qT", kb_)])
                p.op("pool", lambda e, qt=qt, q4t=q4t: e.tensor_copy(q4t[:].rearrange("p (r g i) -> p r g i", r=4, g=4), qt[:].rearrange("p (g i r) -> p r g i", g=4, r=4)),
                     reads=[("qT", kb_)], writes=[("q4", kb_)])
                p.op("pool", lambda e, qt=qt, q16t=q16t: e.tensor_copy(q16t[:].rearrange("p (r i) -> p r i", r=16), qt[:].rearrange("p (i r) -> p r i", r=16)),
                     reads=[("qT", kb_)], writes=[("q16", kb_)])
            h = 2 * hp + hh
            vb = h % 2
            vsrc = VdS[ch * 2048:ch * 2048 + 4096, h * 64:(h + 1) * 64]
            for n4 in range(8):
                p.dma("sp", vA[vb][0][:, 4 * n4:4 * n4 + 4, 0:64], vsrc[n4 * 512:(n4 + 1) * 512, :].rearrange("(n i) c -> i n c", i=128), writes=[("vAv", vb, 0)])
            for g in range(8):
                p.dma("sp", vA[vb][1][:, 4 * g:4 * g + 4, 0:64], vsrc[g * 512:(g + 1) * 512, :].rearrange("(i r) c -> i r c", r=4), writes=[("vAv", vb, 1)])
            for G in range(2):
                v16 = vsrc[G * 2048:(G + 1) * 2048, :].rearrange("(i r) c -> i r c", r=16)
                for r4 in range(4):
                    p.dma("sp", vA[vb][2][:, 16 * G + 4 * r4:16 * G + 4 * r4 + 4, 0:64], v16[:, 4 * r4:4 * r4 + 4, :], writes=[("vAv", vb, 2)])

        def emit_qk(st):
            pre_qk(st)
            ch, hp, hh, br, dil, i = st["ch"], st["hp"], st["hh"], st["br"], st["dil"], st["i"]
            kb_ = (ch * 4 + hp) % 2
            kt = kT[kb_]
            h = 2 * hp + hh
            rows = slice(64 * hh, 64 * hh + 64)
            qkey = {1: ("qT", kb_), 4: ("q4", kb_), 16: ("q16", kb_)}[dil]
            bank = 4 + i % 4
            for slot, (kc, qsrc, qc_, N, bmc, n, oc) in enumerate(st["pair"]):
                so = ps[:, bank, slot * 256:slot * 256 + N]
                p.op("pe", lambda e, so=so, kc=kc, qsrc=qsrc, qc_=qc_, kt=kt, rows=rows: e.matmul(so, kt[rows, kc], qsrc[rows, qc_], start=True, stop=False),
                     reads=[("kT", kb_), qkey], writes=[("S", bank)])
                p.op("pe", lambda e, so=so, bmc=bmc, bi=br * 8 + h: e.matmul(so, identb[:], BM[:, bi, bmc], start=False, stop=True),
                     reads=["identb", "BM"], writes=[("S", bank)])

        def emit_rest(st):
            ch, hp, hh, br, dil, i = st["ch"], st["hp"], st["hh"], st["br"], st["dil"], st["i"]
            h = 2 * hp + hh
            vb = h % 2
            bank = 4 + i % 4
            pair = st["pair"]
            pb = Pb[i % 4]
            if len(pair) == 2 and pair[0][3] == 256 and pair[1][3] == 256:
                p.op("act", lambda e, pb=pb, bank=bank: e.activation(pb[:], ps[:, bank, :], AF.Exp), reads=[("S", bank)], writes=[("Pb", i % 4)])
            else:
                for slot, (kc, qsrc, qc_, N, bmc, n, oc) in enumerate(pair):
                    p.op("act", lambda e, pb=pb, bank=bank, slot=slot, N=N: e.activation(pb[:, slot * 256:slot * 256 + N], ps[:, bank, slot * 256:slot * 256 + N], AF.Exp),
                         reads=[("S", bank)], writes=[("Pb", i % 4)])
            if st["first_br"]:
                touched.clear()
            for slot, (kc, qsrc, qc_, N, bmc, n, oc) in enumerate(pair):
                if oc.start // 512 != (oc.stop - 1) // 512:
                    parts = [(oc.start, oc.start + 128, 0), (oc.start + 128, oc.stop, 128)]
                else:
                    parts = [(oc.start, oc.stop, 0)]
                for (o0, o1, po) in parts:
                    ob = o0 // 512
                    st_flag = ob not in touched
                    touched.add(ob)
                    p.op("pe", lambda e, pb=pb, slot=slot, n=n, o0=o0, o1=o1, po=po, vb=vb, br=br, st_flag=st_flag: e.matmul(Oflat[:, o0:o1], vA[vb][br][:, n, :], pb[:, slot * 256 + po:slot * 256 + po + (o1 - o0)], start=st_flag, stop=True, skip_group_check=True),
                         reads=[("Pb", i % 4), ("vAv", vb, br), ("vAc", vb, br)], writes=["O"])
            if st["last_br"]:
                if dil == 1:
                    p.op("dve", lambda e: e.tensor_copy(acc[:], Oflat), reads=["O"], writes=["acc"])
                elif dil == 4:
                    av = acc[:].rearrange("p (g i r) -> p r g i", g=4, r=4)
                    p.op("dve", lambda e, av=av: e.tensor_tensor(av, av, Oflat.rearrange("p (r g i) -> p r g i", r=4, g=4), ALU.add), reads=["O", "acc"], writes=["acc"])
                else:
                    av = acc[:].rearrange("p (i r) -> p r i", r=16)
                    p.op("dve", lambda e, av=av: e.tensor_tensor(av, av, Oflat.rearrange("p (r i) -> p r i", r=16), ALU.add), reads=["O", "acc"], writes=["acc"])
                    ob_ = onb[h % 2]
                    p.op("dve", lambda e: e.reciprocal(rd0[0:64, :], acc[64:128, :]), reads=["acc"], writes=["rd0"])
                    p.op("dve", lambda e, ob_=ob_: e.tensor_tensor(ob_[0:64, :], acc[0:64, :], rd0[0:64, :], ALU.mult), reads=["acc", "rd0"], writes=[("onb", h % 2)])
                    p.dma("pool", OcS[h * 64:(h + 1) * 64, ch * 2048:(ch + 1) * 2048], ob_[0:64, :], reads=[("onb", h % 2)])
                    if h == 7 and ch == 0 and NCH > 1:
                        for vb2 in range(2):
                            for b3 in range(3):
                                p.op("pool", lambda e, vb2=vb2, b3=b3: e.memset(vA[vb2][b3][:, 0:16, 64:128], 1.0), writes=[("vAc", vb2, b3)])

        if PIPE_B:
            emit_qk(steps[0])
            for i, st in enumerate(steps):
                if i + 1 < len(steps):
                    emit_qk(steps[i + 1])
                emit_rest(st)
        else:
            for i, st in enumerate(steps):
                emit_qk(st)
                emit_rest(st)

    phase_B()
    p.barrier()
    sb.reset(persist_mark)

    def phase_C():
        kT = [sb.alloc(f"kfT{i}", [128, NTOK], BF16) for i in range(2)]
        qT = [sb.alloc(f"qfT{i}", [128, NOWN], BF16) for i in range(2)]
        vA = [sb.alloc(f"vfA{i}", [128, NT, 128], BF16) for i in range(2)]
        biasq = [sb.alloc(f"biasq{i}", [128, NT], F32) for i in range(2)]
        Pb = [sb.alloc(f"Pf{i}", [128, 1024], BF16) for i in range(3)]
        rd0 = sb.alloc("rdf", [128, 1024], F32)
        onb = [sb.alloc(f"onf{i}", [128, 1024], BF16) for i in range(2)]
        for i in range(2):
            p.op("pool", lambda e, i=i: e.memset(kT[i][64:96, :], 1.0), writes=[("kfa", i)])
            p.op("pool", lambda e, i=i: e.tensor_copy(vA[i][:, 0:NTP, 64:128], pvb[:].unsqueeze(1).broadcast_to([128, NTP, 64])), reads=["pvb"], writes=[("vfc", i)])
            p.op("pool", lambda e, i=i: e.memset(vA[i][:, NTP:NT, 64:128], 1.0), writes=[("vfc", i)])
        kcnt = [0]
        wstage = [sb.alloc(f"wstage{i}", [128, 6144], BF16) for i in range(2)]

        def prep_expert(ex):
            b_ = ex % 2
            ws = wstage[b_]
            p.dma("pool", ws[:, 0:2048].rearrange("p (c n) -> p c n", c=KC), w_eg[ex].rearrange("(c p) n -> p c n", p=128), writes=[("wst", b_)])
            p.dma("pool", ws[:, 2048:4096].rearrange("p (c n) -> p c n", c=KC), w_eu[ex].rearrange("(c p) n -> p c n", p=128), writes=[("wst", b_)])
            p.dma("pool", ws[:, 4096:6144].rearrange("p (c n) -> p c n", c=2), w_ed[ex].rearrange("(c p) n -> p c n", p=128), writes=[("wst", b_)])
            p.dma("pool", WALL[ex * 128:(ex + 1) * 128, :], ws[:], reads=[("wst", b_)])

        def head_loads(h):
            hb_ = h % 2
            kt, qt, va = kT[hb_], qT[hb_], vA[hb_]
            p.dma("sp", kt[0:64, :], KfT[h * 64:(h + 1) * 64, :], writes=[("kf", hb_)])
            p.dma("sp", qt[0:64, :], QfT[h * 64:(h + 1) * 64, :], writes=[("qf", hb_)])
            p.dma("sp", qt[64:67, :], QfA[h, :, :], writes=[("qf", hb_)])
            for t8 in range(0, NT, 4):
                p.dma("sp", va[:, t8:t8 + 4, 0:64], VfS[t8 * 128:(t8 + 4) * 128, h * 64:(h + 1) * 64].rearrange("(n i) c -> i n c", i=128),
                      writes=[("vf", hb_)])

        steps = []
        oi = 0
        for h in range(8):
            for qc in range(NQC):
                nfull = (NPREV + 1024 * qc) // 128
                total = nfull + 8
                for kb in range(total):
                    steps.append(dict(h=h, qc=qc, kb=kb, nfull=nfull, total=total, oi=oi, first=(kb == 0), last=(kb == total - 1)))
                oi += 1
        for i, st in enumerate(steps):
            st["ki"] = i

        def halves(c0):
            res = []
            for half in range(2):
                lo, hi = max(c0, 512 * half), 512 * (half + 1)
                if lo < hi:
                    res.append((half, lo, hi))
            return res

        def emit_qk(st):
            h, qc, kb, ki = st["h"], st["qc"], st["kb"], st["ki"]
            hb_ = h % 2
            kt, qt = kT[hb_], qT[hb_]
            if st["first"]:
                if qc == 0 and h == 0:
                    head_loads(0)
                bq = biasq[st["oi"] % 2]
                total = st["total"]
                p.op("dve", lambda e, bq=bq, h=h, qc=qc, total=total: e.tensor_scalar(bq[:, 0:total], ctok[:, 0:total, h], -1.0, crefbc[:, qc, h:h + 1], ALU.mult, ALU.add),
                     reads=["ctok", "crefbc"], writes=[("biasq", st["oi"] % 2)])
            sb0 = 4 + 2 * (ki % 2)
            Sk = ("Sf", ki % 2)
            j = kb - st["nfull"]
            c0 = 128 * j if j >= 0 else 0
            for (half, lo, hi) in halves(c0):
                diag_here = (j >= 0 and lo == c0)
                p.op("pe", lambda e, sb0=sb0, half=half, lo=lo, hi=hi, kb=kb, qc=qc, kt=kt, qt=qt, diag_here=diag_here:
                     e.matmul(ps[:, sb0 + half, lo - 512 * half:512], kt[0:67, kb * 128:(kb + 1) * 128], qt[0:67, qc * 1024 + lo:qc * 1024 + hi], start=True, stop=not diag_here),
                     reads=[("kf", hb_), ("kfa", hb_), ("qf", hb_)], writes=[Sk])
                if diag_here:
                    p.op("pe", lambda e, sb0=sb0, half=half, lo=lo: e.matmul(ps[:, sb0 + half, lo - 512 * half:lo - 512 * half + 128], identb[:], mneg[:], start=False, stop=True),
                         reads=["identb", "mneg"], writes=[Sk])

        def emit_rest(st):
            h, qc, kb, ki, total = st["h"], st["qc"], st["kb"], st["ki"], st["total"]
            hb_ = h % 2
            va = vA[hb_]
            bq = biasq[st["oi"] % 2]
            bqk = ("biasq", st["oi"] % 2)
            sb0 = 4 + 2 * (ki % 2)
            Sk = ("Sf", ki % 2)
            pbi = ki % 3
            pb = Pb[pbi]
            ob0 = 2 * (st["oi"] % 2)
            Ok = ("Of", st["oi"] % 2)
            j = kb - st["nfull"]
            c0 = 128 * j if j >= 0 else 0
            Sflat = psb(sb0, 2)
            if st["first"] and qc == 0 and h + 1 < 8:
                head_loads(h + 1)
            p.op("act", lambda e, pb=pb, Sflat=Sflat, c0=c0, bq=bq, kb=kb: e.activation(pb[:, c0:1024], Sflat[:, c0:1024], AF.Exp, bias=bq[:, kb:kb + 1]),
                 reads=[Sk, bqk], writes=[("Pf", pbi)])
            for (half, lo, hi) in halves(c0):
                p.op("pe", lambda e, ob0=ob0, half=half, lo=lo, hi=hi, kb=kb, va=va, pb=pb, total=total:
                     e.matmul(ps[:, ob0 + half, lo - 512 * half:512], va[:, kb, :], pb[:, lo:hi], start=(kb == 0), stop=(kb == total - 1), skip_group_check=True),
                     reads=[("Pf", pbi), ("vf", hb_), ("vfc", hb_)], writes=[Ok])
            if st["last"]:
                Of = psb(ob0, 2)
                ob = onb[st["oi"] % 2]
                p.op("dve", lambda e, Of=Of: e.reciprocal(rd0[0:64, :], Of[64:128, :]), reads=[Ok], writes=["rdf"])
                p.op("dve", lambda e, Of=Of, ob=ob: e.tensor_tensor(ob[0:64, :], Of[0:64, :], rd0[0:64, :], ALU.mult), reads=[Ok, "rdf"], writes=[("onf", st["oi"] % 2)])
                p.dma("pool", OcS[512 + h * 64:512 + (h + 1) * 64, qc * 1024:(qc + 1) * 1024], ob[0:64, :], reads=[("onf", st["oi"] % 2)])

        prep_at = {}
        for ex in range(NEXP):
            prep_at.setdefault(min(len(steps) - 1, (ex * len(steps)) // NEXP), []).append(ex)
        if PIPE_C:
            emit_qk(steps[0])
            for i, st in enumerate(steps):
                if i + 1 < len(steps):
                    emit_qk(steps[i + 1])
                emit_rest(st)
                for ex in prep_at.get(i, []):
                    if not NO_WALL:
                        prep_expert(ex)
        else:
            for i, st in enumerate(steps):
                emit_qk(st)
                emit_rest(st)

    phase_C()
    p.barrier()
    sb.reset(persist_mark)

    def phase_D():
        T_ = NTO
        E_ = NEXP
        NTL = 2 * NOWN // 128 + 32
        NS = NTL * 128
        wo = sb.alloc("wo", [128, 8, D], BF16)
        gcat = sb.alloc("gcat", [128, 8], F32)
        wr = sb.alloc("wr", [128, KC, 36], BF16)
        brb = sb.alloc("brb", [128, 36], F32)
        g2bc = sb.alloc("g2bc", [128, D], F32)
        gfbc = sb.alloc("gfbc", [128, D], F32)
        tris = sb.alloc("tris", [128, 128], F32)
        onesf = sb.alloc("onesfD", [128, 128], F32)
        tokid = sb.alloc("tokid", [128, T_], F32)
        jv = sb.alloc("jv", [128, NTL], F32)
        pidx = sb.alloc("pidx", [128, 1], F32)
        Lg = sb.alloc("Lg", [128, T_, 36], F32)
        A1 = sb.alloc("A1", [128, T_, E_], F32)
        A2 = sb.alloc("A2", [128, T_, E_], F32)
        wk = sb.alloc("wk", [128, T_, 2], F32)
        widx = sb.alloc("widx", [128, NTL], I32)
        junk = sb.alloc("junkD", [128, D], BF16)
        mX = sb.mark()
        wof = sb.alloc("wof", [128, 8, D], F32)
        p.dma("sp", wof[:], w_out.rearrange("(b p) n -> p b n", p=128), writes=["wof"])
        p.dma("sp", gcat[:, 0:4], out_norm_dil.rearrange("(b p) -> p b", p=128), writes=["gcat"], allow_slow_non_contiguous=True)
        p.dma("sp", gcat[:, 4:8], out_norm_fox.rearrange("(b p) -> p b", p=128), writes=["gcat"], allow_slow_non_contiguous=True)
        for b in range(8):
            eng = "dve" if b % 2 == 0 else "pool"
            p.op(eng, lambda e, b=b: e.tensor_scalar(wo[:, b, :], wof[:, b, :], gcat[:, b:b + 1], None, ALU.mult), reads=["wof", "gcat"], writes=[("wo", b)])
        p.dma("pool", wr[:, :, 0:4], w_rg.rearrange("(c p) n -> p c n", p=128), writes=["wr"])
        p.dma("pool", wr[:, :, 4:36], w_re.rearrange("(c p) n -> p c n", p=128), writes=["wr"])
        p.dma("sp", brb[:, 0:4], b_rg.partition_broadcast(128), writes=["brb"])
        p.dma("sp", brb[:, 4:36], b_re.partition_broadcast(128), writes=["brb"])
        p.dma("sp", g2bc[:], ffn_norm.partition_broadcast(128), writes=["g2bc"])
        p.dma("sp", gfbc[:], final_norm.partition_broadcast(128), writes=["gfbc"])
        p.dma("sp", tris[:], tris_d[:, :], writes=["tris"])
        p.dma("sp", tokid[:], tokid_d[:, :], writes=["tokid"])
        p.dma("sp", jv[:], jv_d[:, :], writes=["jv"])
        p.dma("sp", pidx[:], pidx_d[:, :], writes=["pidx"])
        p.dma("sp", SLOTINFO.rearrange("(p r) c -> p (r c)", p=128), slot_init_d.rearrange("(p r) c -> p (r c)", p=128), writes=["slotinfo"])
        p.op("pool", lambda e: e.memset(onesf[:], 1.0), writes=["onesfD"])

        OcT = [sb.alloc(f"OcT{i}", [128, 8, 512], BF16) for i in range(2)]
        xo = [sb.alloc(f"xo{i}", [128, D], F32) for i in range(3)]
        NX1 = 6
        x1 = [sb.alloc(f"x1{i}", [128, D], F32) for i in range(NX1)]
        sq8 = [sb.alloc(f"sq8{i}", [128, 8, 128], BF16) for i in range(2)]
        h2b = [sb.alloc(f"h2b{i}", [128, D], BF16) for i in range(3)]
        h2T = [sb.alloc(f"h2T{i}", [128, KC, 128], BF16) for i in range(2)]
        st1 = sb.alloc("st1", [128, T_, 2], F32)
        st2 = sb.alloc("st2", [128, T_], F32)
        p.op("dve", lambda e: e.memset(st2[:], 0.0), writes=["st2z"])
        for g4 in range(T_ // 4):
            oc = OcT[g4 % 2]
            ock = ("OcT", g4 % 2)
            p.dma("sp", oc[:], OcS[:, g4 * 512:(g4 + 1) * 512].rearrange("(b p) t -> p b t", p=128), writes=[ock])
            for i in range(4):
                tt = 4 * g4 + i
                ts_ = slice(i * 128, (i + 1) * 128)
                s8 = sq8[tt % 2]
                p.op("pool", lambda e, s8=s8, oc=oc, ts_=ts_: e.tensor_tensor(s8[:], oc[:, :, ts_], oc[:, :, ts_], ALU.mult), reads=[ock], writes=[("sq8", tt % 2)])
                for grp in range(2):
                    for b in range(4):
                        p.op("pe", lambda e, s8=s8, grp=grp, b=b, i=i: e.matmul(ps[:, 7, 2 * i + grp:2 * i + grp + 1], s8[:, 4 * grp + b, :], ones_bf[:], start=(b == 0), stop=(b == 3), skip_group_check=True),
                             reads=[("sq8", tt % 2), "ones_bf"], writes=[("bk", 7)])
            gs = slice(4 * g4, 4 * g4 + 4)
            st1g = st1[:, gs, :].rearrange("p t k -> p (t k)")
            p.op("dve", lambda e, st1g=st1g: e.tensor_scalar(st1g, ps[:, 7, 0:8], 1.0 / 512, EPS, ALU.mult, ALU.add), reads=[("bk", 7)], writes=[("st1", g4)])
            p.op("act", lambda e, st1g=st1g: e.activation(st1g, st1g, AF.Ln), reads=[("st1", g4)], writes=[("st1", g4)])
            p.op("act", lambda e, st1g=st1g: e.activation(st1g, st1g, AF.Exp, scale=-0.5), reads=[("st1", g4)], writes=[("st1", g4)])
            for i in range(4):
                tt = 4 * g4 + i
                ts_ = slice(i * 128, (i + 1) * 128)
                xb = xo[tt % 3]
                x1t = x1[tt % NX1]
                p.dma("sp", xb[:], xs[NPREV + tt * 128:NPREV + (tt + 1) * 128, :], writes=[("xo", tt % 3)])
                for grp in range(2):
                    for half in range(2):
                        for b in range(4):
                            p.op("pe", lambda e, grp=grp, half=half, b=b, ts_=ts_, oc=oc: e.matmul(ps[:, 2 * grp + half, :], oc[:, 4 * grp + b, ts_], wo[:, 4 * grp + b, half * 512:(half + 1) * 512], start=(b == 0), stop=(b == 3)),
                                 reads=[ock, ("wo", 4 * grp + b)], writes=[("bk", 2 * grp + half)])
                p.op("dve", lambda e, x1t=x1t, xb=xb, tt=tt: e.scalar_tensor_tensor(x1t[:], psb(0, 2), st1[:, tt, 0:1], xb[:], ALU.mult, ALU.add),
                     reads=[("bk", 0), ("bk", 1), ("st1", g4), ("xo", tt % 3)], writes=[("x1", tt % NX1)])
                p.op("dve", lambda e, x1t=x1t, tt=tt: e.scalar_tensor_tensor(x1t[:], psb(2, 2), st1[:, tt, 1:2], x1t[:], ALU.mult, ALU.add),
                     reads=[("bk", 2), ("bk", 3), ("st1", g4), ("x1", tt % NX1)], writes=[("x1", tt % NX1)])
                p.dma("pool", X1S[tt * 128:(tt + 1) * 128, :], x1t[:], reads=[("x1", tt % NX1)])
                if debug:
                    finals.append(p.dma("pool", dbg["d_x1"][tt * 128:(tt + 1) * 128, :], x1t[:], reads=[("x1", tt % NX1)]))
                p.op("act", lambda e, x1t=x1t, tt=tt: e.activation(junk[:], x1t[:], AF.Square, accum_out=st2[:, tt:tt + 1]), reads=[("x1", tt % NX1), "st2z"], writes=["junkD", ("ss2", tt)])
            st2g = st2[:, gs]
            p.op("dve", lambda e, st2g=st2g: e.tensor_scalar(st2g, st2g, 1.0 / D, EPS, ALU.mult, ALU.add), reads=[("ss2", 4 * g4 + i) for i in range(4)], writes=[("st2", g4)])
            p.op("act", lambda e, st2g=st2g: e.activation(st2g, st2g, AF.Ln), reads=[("st2", g4)], writes=[("st2", g4)])
            p.op("act", lambda e, st2g=st2g: e.activation(st2g, st2g, AF.Exp, scale=-0.5), reads=[("st2", g4)], writes=[("st2", g4)])
            for i in range(4):
                tt = 4 * g4 + i
                x1t = x1[tt % NX1]
                hbt = h2b[tt % 3]
                p.op("dve", lambda e, hbt=hbt, x1t=x1t, tt=tt: e.scalar_tensor_tensor(hbt[:], x1t[:], st2[:, tt:tt + 1], g2bc[:], ALU.mult, ALU.mult),
                     reads=[("x1", tt % NX1), ("st2", g4), "g2bc"], writes=[("h2b", tt % 3)])
                p.dma("pool", H2S[tt * 128:(tt + 1) * 128, :], hbt[:], reads=[("h2b", tt % 3)])
                pt = ps[:, 4 + tt % 2, :].bitcast(BF16)
                for c in range(KC):
                    p.op("pe", lambda e, pt=pt, hbt=hbt, c=c: e.transpose(pt[:, c * 128:(c + 1) * 128], hbt[:, c * 128:(c + 1) * 128], identb[:]),
                         reads=[("h2b", tt % 3), "identb"], writes=[("bk", 4 + tt % 2)])
                hT = h2T[tt % 2]
                p.op("act", lambda e, pt=pt, hT=hT: e.copy(hT[:], pt.rearrange("p (c t) -> p c t", c=KC)), reads=[("bk", 4 + tt % 2)], writes=[("h2T", tt % 2)])
                lp = ps[:, 6, (tt % 8) * 36:(tt % 8) * 36 + 36]
                for c in range(KC):
                    p.op("pe", lambda e, c=c, hT=hT, lp=lp: e.matmul(lp, hT[:, c, :], wr[:, c, :], start=(c == 0), stop=(c == KC - 1), skip_group_check=True),
                         reads=[("h2T", tt % 2), "wr"], writes=[("bk", 6)])
                p.op("dve", lambda e, lp=lp, tt=tt: e.tensor_tensor(Lg[:, tt, :], lp, brb[:], ALU.add), reads=[("bk", 6), "brb"], writes=["Lg"])

        if SKIP_D1B:
            p.barrier()
            return
        rk = "Lg"
        R = sb.alloc("Rr", [128, T_, 16], F32)
        Lm = sb.alloc("Lm", [128, T_, E_], F32)
        tmpE = sb.alloc("tmpE", [128, T_, E_], F32)

        def bc(ap2, n):
            return ap2.unsqueeze(2).broadcast_to([128, T_, n])

        Lgrp = Lg[:, :, 0:4]
        Lexp = Lg[:, :, 4:36]
        p.op("dve", lambda e: e.tensor_reduce(R[:, :, 0], Lgrp, AX.X, ALU.max), reads=[rk], writes=[rk])
        p.op("dve", lambda e: e.tensor_tensor(R[:, :, 4:8], Lgrp, bc(R[:, :, 0], 4), ALU.subtract), reads=[rk], writes=[rk])
        p.op("act", lambda e: e.activation(R[:, :, 8:12], R[:, :, 4:8], AF.Exp), reads=[rk], writes=[rk])
        p.op("dve", lambda e: e.tensor_reduce(R[:, :, 1], R[:, :, 8:12], AX.X, ALU.add), reads=[rk], writes=[rk])
        p.op("dve", lambda e: e.reciprocal(R[:, :, 1], R[:, :, 1]), reads=[rk], writes=[rk])
        p.op("dve", lambda e: e.tensor_tensor(R[:, :, 4:8], Lgrp, bc(R[:, :, 0], 4), ALU.is_equal), reads=[rk], writes=[rk])
        p.op("dve", lambda e: e.tensor_scalar(R[:, :, 4:8], R[:, :, 4:8], 1.0, 1e30, ALU.subtract, ALU.mult), reads=[rk], writes=[rk])
        p.op("dve", lambda e: e.tensor_tensor(Lm[:].rearrange("p t (g j) -> p t g j", g=4), Lexp.rearrange("p t (g j) -> p t g j", g=4),
                                              R[:, :, 4:8].unsqueeze(3).broadcast_to([128, T_, 4, 8]), ALU.add), reads=[rk], writes=[rk])
        p.op("dve", lambda e: e.tensor_reduce(R[:, :, 2], Lm[:], AX.X, ALU.max), reads=[rk], writes=[rk])
        p.op("dve", lambda e: e.tensor_tensor(A1[:], Lm[:], bc(R[:, :, 2], E_), ALU.is_equal), reads=[rk], writes=[rk])
        p.op("dve", lambda e: e.scalar_tensor_tensor(Lm[:], A1[:], -1e30, Lm[:], ALU.mult, ALU.add), reads=[rk], writes=[rk])
        p.op("dve", lambda e: e.tensor_reduce(R[:, :, 3], Lm[:], AX.X, ALU.max), reads=[rk], writes=[rk])
        p.op("dve", lambda e: e.tensor_tensor(A2[:], Lm[:], bc(R[:, :, 3], E_), ALU.is_equal), reads=[rk], writes=[rk])
        p.op("dve", lambda e: e.tensor_tensor(R[:, :, 12], R[:, :, 3], R[:, :, 2], ALU.subtract), reads=[rk], writes=[rk])
        p.op("act", lambda e: e.activation(R[:, :, 12], R[:, :, 12], AF.Exp), reads=[rk], writes=[rk])
        p.op("dve", lambda e: e.tensor_scalar(R[:, :, 13], R[:, :, 12], 1.0, None, ALU.add), reads=[rk], writes=[rk])
        p.op("dve", lambda e: e.reciprocal(R[:, :, 13], R[:, :, 13]), reads=[rk], writes=[rk])
        p.op("dve", lambda e: e.tensor_tensor(R[:, :, 14], R[:, :, 12], R[:, :, 13], ALU.mult), reads=[rk], writes=[rk])
        p.op("dve", lambda e: e.tensor_tensor(wk[:, :, 0], R[:, :, 13], R[:, :, 1], ALU.mult), reads=[rk], writes=[rk])
        p.op("dve", lambda e: e.tensor_tensor(wk[:, :, 1], R[:, :, 14], R[:, :, 1], ALU.mult), reads=[rk], writes=[rk])
        if debug:
            cmb = sb.alloc("cmb", [128, T_, E_], F32)
            p.op("dve", lambda e: e.tensor_tensor(cmb[:], A1[:], bc(wk[:, :, 0], E_), ALU.mult), reads=[rk], writes=["cmb"])
            p.op("dve", lambda e: e.tensor_tensor(tmpE[:], A2[:], bc(wk[:, :, 1], E_), ALU.mult), reads=[rk], writes=["tmpE"])
            p.op("dve", lambda e: e.tensor_tensor(cmb[:], cmb[:], tmpE[:], ALU.add), reads=["cmb", "tmpE"], writes=["cmb"])
            finals.append(p.dma("sp", dbg["d_comb"].rearrange("(t p) e -> p t e", p=128), cmb[:], reads=["cmb"]))

        if SKIP_D2:
            p.barrier()
            return
        NF = T_ * E_
        Aall = sb.alloc("Aall", [128, NF], F32)
        Wsb = sb.alloc("Wsb", [128, NF], F32)
        tA = sb.alloc("tA2", [128, NF], F32)
        tB = sb.alloc("tB2", [128, NF], F32)
        Tot = sb.alloc("Tot", [128, NF], F32)
        A1f = A1[:].rearrange("p t e -> p (t e)")
        A2f = A2[:].rearrange("p t e -> p (t e)")
        p.op("dve", lambda e: e.tensor_tensor(Aall[:], A1f, A2f, ALU.add), reads=[rk], writes=["Aall"])
        nbk = NF // 512
        assert nbk <= 2
        for bk in range(nbk):
            cs = slice(bk * 512, (bk + 1) * 512)
            p.op("pe", lambda e, cs=cs, bk=bk: e.matmul(ps[:, bk, :], tris[:], Aall[:, cs], start=True, stop=True), reads=["tris", "Aall"], writes=[("bk", bk)])
            p.op("pe", lambda e, cs=cs, bk=bk: e.matmul(ps[:, 2 + bk, :], onesf[:], Aall[:, cs], start=True, stop=True), reads=["onesfD", "Aall"], writes=[("bk", 2 + bk)])
            p.op("dve", lambda e, cs=cs, bk=bk: e.tensor_copy(Wsb[:, cs], ps[:, bk, :]), reads=[("bk", bk)], writes=["Wsb"])
            p.op("act", lambda e, cs=cs, bk=bk: e.copy(tA[:, cs], ps[:, 2 + bk, :]), reads=[("bk", 2 + bk)], writes=["tAB"])
            p.op("act", lambda e, cs=cs, bk=bk: e.copy(Tot[:, cs], ps[:, 2 + bk, :]), reads=[("bk", 2 + bk)], writes=["Tot"])
        src, dst = tA, tB
        k = 1
        while k < T_:
            kk = k * E_
            p.op("dve", lambda e, src=src, dst=dst, kk=kk: e.tensor_copy(dst[:, 0:kk], src[:, 0:kk]), reads=["tAB"], writes=["tAB"])
            p.op("dve", lambda e, src=src, dst=dst, kk=kk: e.tensor_tensor(dst[:, kk:NF], src[:, kk:NF], src[:, 0:NF - kk], ALU.add), reads=["tAB"], writes=["tAB"])
            src, dst = dst, src
            k *= 2
        if D2_STOP == 1:
            p.barrier()
            return
        incl = src
        cnt_f = sb.alloc("cnt_f", [128, E_], F32)
        cnt_i = sb.alloc("cnt_i", [128, E_], I32)
        psz = sb.alloc("psz", [128, E_], F32)
        eA = sb.alloc("eA", [128, E_], F32)
        eB = sb.alloc("eB", [128, E_], F32)
        base = sb.alloc("base", [128, E_], F32)
        p.op("dve", lambda e: e.tensor_copy(cnt_f[:], incl[:, NF - E_:NF]), reads=["tAB"], writes=["cnt"])
        p.op("dve", lambda e: e.tensor_copy(cnt_i[:], cnt_f[:]), reads=["cnt"], writes=["cnti"])
        p.op("dve", lambda e: e.tensor_scalar(cnt_i[:], cnt_i[:], 127, None, ALU.add), reads=["cnti"], writes=["cnti"])
        p.op("dve", lambda e: e.tensor_scalar(cnt_i[:], cnt_i[:], 7, 7, ALU.arith_shift_right, ALU.logical_shift_left), reads=["cnti"], writes=["cnti"])
        p.op("dve", lambda e: e.tensor_copy(psz[:], cnt_i[:]), reads=["cnti"], writes=["psz"])
        if D2_STOP == 2:
            p.barrier()
            return
        p.op("dve", lambda e: e.tensor_copy(eA[:], psz[:]), reads=["psz"], writes=["eAB"])
        src2, dst2 = eA, eB
        k = 1
        HS_MAX = int(_os.environ.get('HS_MAX', '64'))
        while k < min(E_, HS_MAX):
            p.op("dve", lambda e, src2=src2, dst2=dst2, k=k: e.tensor_copy(dst2[:, 0:k], src2[:, 0:k]), reads=["eAB"], writes=["eAB"])
            p.op("dve", lambda e, src2=src2, dst2=dst2, k=k: e.tensor_tensor(dst2[:, k:E_], src2[:, k:E_], src2[:, 0:E_ - k], ALU.add), reads=["eAB"], writes=["eAB"])
            src2, dst2 = dst2, src2
            k *= 2
        endp = src2
        p.op("dve", lambda e: e.tensor_tensor(base[:], endp[:], psz[:], ALU.subtract), reads=["eAB", "psz"], writes=["base"])
        if D2_STOP == 3:
            p.barrier()
            return
        p.op("dve", lambda e: e.tensor_tensor(Wsb[:], Wsb[:], incl[:], ALU.add), reads=["Wsb", "tAB"], writes=["Wsb"])
        p.op("dve", lambda e: e.tensor_tensor(Wsb[:], Wsb[:], Tot[:], ALU.subtract), reads=["Wsb", "Tot"], writes=["Wsb"])
        W3 = Wsb[:].rearrange("p (t e) -> p t e", e=E_)
        p.op("dve", lambda e: e.tensor_tensor(W3, W3, base[:].unsqueeze(1).broadcast_to([128, T_, E_]), ALU.add), reads=["Wsb", "base"], writes=["Wsb"])
        slotf = sb.alloc("slotf", [128, T_, 2], F32)
        sl_i = sb.alloc("sl_i", [128, T_, 2], I32)
        for k_, Ak in enumerate((A1, A2)):
            p.op("dve", lambda e, Ak=Ak: e.tensor_tensor(tmpE[:], W3, Ak[:], ALU.mult), reads=["Wsb", rk], writes=["tmpE"])
            p.op("dve", lambda e, k_=k_: e.tensor_reduce(slotf[:, :, k_], tmpE[:], AX.X, ALU.add), reads=["tmpE"], writes=["slotf"])
        p.op("dve", lambda e: e.tensor_copy(sl_i[:], slotf[:]), reads=["slotf"], writes=["sl_i"])
        if debug:
            finals.append(p.dma("sp", dbg["d_slot"][:, :], slotf[:].rearrange("p t k -> p (t k)"), reads=["slotf"], writes=["dslot"]))
            finals.append(p.dma("sp", dbg["d_cnt"][:, 0:32], cnt_f[:], reads=["cnt"]))
            finals.append(p.dma("sp", dbg["d_cnt"][:, 32:64], psz[:], reads=["psz"]))
            finals.append(p.dma("sp", dbg["d_cnt"][:, 64:96], base[:], reads=["base"]))
        if D2_STOP == 4:
            p.barrier()
            return
        info = sb.alloc("info", [128, T_, 2, 4], I32)
        info_f = info[:].bitcast(F32)
        p.op("dve", lambda e: e.memset(info[:], 0), writes=["info"])
        for k_ in range(2):
            p.op("dve", lambda e, k_=k_: e.tensor_copy(info[:, :, k_, 0], tokid[:]), reads=["tokid", "info"], writes=["info"])
            p.op("dve", lambda e, k_=k_: e.tensor_scalar(slotf[:, :, k_], tokid[:], float(k_ * NOWN), None, ALU.add), reads=["tokid", "sl_i", "dslot"], writes=["slotf2"])
            p.op("dve", lambda e, k_=k_: e.tensor_copy(info[:, :, k_, 1], slotf[:, :, k_]), reads=["slotf2", "info"], writes=["info"])
            p.op("dve", lambda e, k_=k_: e.tensor_copy(info_f[:, :, k_, 2], wk[:, :, k_]), reads=[rk, "info"], writes=["info"])
        if not SKIP_D3:
            for tt in range(T_):
                for k_ in range(2):
                    p.dma_custom("pool", lambda e, tt=tt, k_=k_: e.indirect_dma_start(out=SLOTINFO[:, :], out_offset=bass.IndirectOffsetOnAxis(ap=sl_i[:, tt, k_:k_ + 1], axis=0),
                                                                                         in_=info[:, tt, k_, :], in_offset=None),
                                 reads=["info", "sl_i", "slotinfo"], writes=[("slotw", tt, k_)])
        if D2_STOP == 5:
            p.barrier()
            return
        cmpT = sb.alloc("cmpT", [128, NTL, E_], F32)
        eidf = sb.alloc("eidf", [128, NTL], F32)
        p.op("dve", lambda e: e.tensor_tensor(cmpT[:], endp[:].unsqueeze(1).broadcast_to([128, NTL, E_]), jv[:].unsqueeze(2).broadcast_to([128, NTL, E_]), ALU.is_le),
             reads=["eAB", "jv"], writes=["cmpT"])
        p.op("dve", lambda e: e.tensor_reduce(eidf[:], cmpT[:], AX.X, ALU.add), reads=["cmpT"], writes=["eidf"])
        p.op("dve", lambda e: e.tensor_scalar(eidf[:], eidf[:], float(E_ - 1), 128.0, ALU.min, ALU.mult), reads=["eidf"], writes=["eidf"])
        p.op("dve", lambda e: e.tensor_scalar(eidf[:], eidf[:], pidx[:, 0:1], None, ALU.add), reads=["eidf", "pidx"], writes=["eidf"])
        p.op("dve", lambda e: e.tensor_copy(widx[:], eidf[:]), reads=["eidf"], writes=["widx"])
        if debug:
            finals.append(p.dma("sp", dbg["d_eid"][:, :], eidf[:], reads=["eidf"]))
        if SKIP_D3:
            p.barrier()
            return
        p.barrier()

        sb.reset(mX)
        NB3 = 6
        LOOK = 3
        NBI = 16
        infoj = [sb.alloc(f"infoj{i}", [128, 4], I32) for i in range(NBI)]
        hg = [sb.alloc(f"hg{i}", [128, D], BF16) for i in range(NB3)]
        wt = [sb.alloc(f"wt{i}", [128, 6144], BF16) for i in range(NB3)]
        hgT = [sb.alloc(f"hgT{i}", [128, KC, 128], BF16) for i in range(2)]
        sg = [sb.alloc(f"sg{i}", [128, DEXP], F32) for i in range(2)]
        he = [sb.alloc(f"he{i}", [128, DEXP], BF16) for i in range(3)]
        heT = [sb.alloc(f"heT{i}", [128, 2, 128], BF16) for i in range(2)]
        ysb = [sb.alloc(f"ysb{i}", [128, D], F32) for i in range(4)]
        ycnt = [0]

        def t_load(j):
            b3 = j % NB3
            bi = j % NBI
            p.dma("sp", infoj[bi][:], SLOTINFO[j * 128:(j + 1) * 128, :], writes=[("infoj", bi)])
            p.dma_custom("pool", lambda e, b3=b3, bi=bi: e.indirect_dma_start(out=hg[b3][:], out_offset=None, in_=H2S[:, :], in_offset=bass.IndirectOffsetOnAxis(ap=infoj[bi][:, 0:1], axis=0)),
                         reads=[("infoj", bi)], writes=[("hg", b3)])
            p.dma_custom("pool", lambda e, b3=b3, j=j: e.indirect_dma_start(out=wt[b3][:], out_offset=None, in_=WALL[:, :], in_offset=bass.IndirectOffsetOnAxis(ap=widx[:, j:j + 1], axis=0)),
                         reads=["widx"], writes=[("wt", b3)])

        def st_T(j):
            b3, b2 = j % NB3, j % 2
            pt = ps[:, b2, :].bitcast(BF16)
            for c in range(KC):
                p.op("pe", lambda e, pt=pt, c=c, b3=b3: e.transpose(pt[:, c * 128:(c + 1) * 128], hg[b3][:, c * 128:(c + 1) * 128], identb[:]),
                     reads=[("hg", b3), "identb"], writes=[("bk", b2)])
            p.op("act", lambda e, pt=pt, b2=b2: e.copy(hgT[b2][:], pt.rearrange("p (c t) -> p c t", c=KC)), reads=[("bk", b2)], writes=[("hgT", b2)])

        def st_G(j):
            b3, b2, h3 = j % NB3, j % 2, j % 3
            gu = ps[:, 2 + b2, :]
            for w0 in (0, 2048):
                for c in range(KC):
                    p.op("pe", lambda e, gu=gu, w0=w0, c=c, b3=b3, b2=b2: e.matmul(gu[:, (w0 // 2048) * 256:(w0 // 2048) * 256 + 256], hgT[b2][:, c, :], wt[b3][:, w0 + c * 256:w0 + (c + 1) * 256], start=(c == 0), stop=(c == KC - 1), skip_group_check=True),
                         reads=[("hgT", b2), ("wt", b3)], writes=[("bk", 2 + b2)])
            p.op("act", lambda e, gu=gu, b2=b2: e.activation(sg[b2][:], gu[:, 0:256], AF.Silu), reads=[("bk", 2 + b2)], writes=[("sg", b2)])
            bi = j % NBI
            wcol = infoj[bi][:].bitcast(F32)[:, 2:3]
            p.op("dve", lambda e, gu=gu, b2=b2, h3=h3, wcol=wcol: e.scalar_tensor_tensor(he[h3][:], gu[:, 256:512], wcol, sg[b2][:], ALU.mult, ALU.mult),
                 reads=[("bk", 2 + b2), ("sg", b2), ("infoj", bi)], writes=[("he", h3)])

        def st_H(j):
            b2, h3 = j % 2, j % 3
            pt = ps[:, 4, :].bitcast(BF16)
            for k_ in range(2):
                p.op("pe", lambda e, pt=pt, k_=k_, h3=h3: e.transpose(pt[:, k_ * 128:(k_ + 1) * 128], he[h3][:, k_ * 128:(k_ + 1) * 128], identb[:]),
                     reads=[("he", h3), "identb"], writes=[("bk", 4)])
            p.op("act", lambda e, pt=pt, b2=b2: e.copy(heT[b2][:], pt[:, 0:256].rearrange("p (k t) -> p k t", k=2)), reads=[("bk", 4)], writes=[("heT", b2)])

        def st_Y(j):
            b3, b2, y4 = j % NB3, j % 2, j % 4
            for half in range(2):
                yb = 5 + ycnt[0] % 3
                ycnt[0] += 1
                for k_ in range(2):
                    p.op("pe", lambda e, yb=yb, k_=k_, half=half, b2=b2, b3=b3: e.matmul(ps[:, yb, :], heT[b2][:, k_, :], wt[b3][:, 4096 + k_ * 1024 + half * 512:4096 + k_ * 1024 + (half + 1) * 512], start=(k_ == 0), stop=(k_ == 1)),
                         reads=[("heT", b2), ("wt", b3)], writes=[("bk", yb)])
                if half == 0:
                    p.op("act", lambda e, yb=yb, y4=y4: e.copy(ysb[y4][:, 0:512], ps[:, yb, :]), reads=[("bk", yb)], writes=[("ysb", y4, 0)])
                else:
                    p.op("dve", lambda e, yb=yb, y4=y4: e.tensor_copy(ysb[y4][:, 512:1024], ps[:, yb, :]), reads=[("bk", yb)], writes=[("ysb", y4, 1)])
            bi = j % NBI
            p.dma_custom("pool", lambda e, y4=y4, bi=bi: e.indirect_dma_start(out=YS[:, :], out_offset=bass.IndirectOffsetOnAxis(ap=infoj[bi][:, 1:2], axis=0), in_=ysb[y4][:], in_offset=None),
                         reads=[("ysb", y4, 0), ("ysb", y4, 1), ("infoj", bi)])

        for j in range(min(LOOK, NTL)):
            t_load(j)
        for i in range(NTL + 3):
            if i < NTL:
                st_T(i)
            if 0 <= i - 1 < NTL:
                st_G(i - 1)
            if 0 <= i - 2 < NTL:
                st_H(i - 2)
            if 0 <= i - 3 < NTL:
                st_Y(i - 3)
            if i + LOOK < NTL:
                t_load(i + LOOK)
        p.barrier()

        sb.reset(mX)
        xa = [sb.alloc(f"xa{i}", [128, D], F32) for i in range(6)]
        ya = [sb.alloc(f"ya{i}", [128, D], F32) for i in range(3)]
        yb_ = [sb.alloc(f"yb{i}", [128, D], F32) for i in range(3)]
        outt = [sb.alloc(f"outt{i}", [128, D], F32) for i in range(3)]
        st3 = sb.alloc("st3", [128, T_], F32)
        p.op("dve", lambda e: e.memset(st3[:], 0.0), writes=["st3z"])
        for g4 in range(T_ // 4):
            for i in range(4):
                tt = 4 * g4 + i
                b3 = tt % 3
                b6 = tt % 6
                rs = slice(tt * 128, (tt + 1) * 128)
                p.dma("sp", xa[b6][:], X1S[rs, :], writes=[("xa", b6)])
                p.dma("sp", ya[b3][:], YS[rs, :], writes=[("ya", b3)])
                p.dma("sp", yb_[b3][:], YS[NOWN + tt * 128:NOWN + (tt + 1) * 128, :], writes=[("yb", b3)])
                p.op("pool", lambda e, b3=b3: e.tensor_tensor(ya[b3][:], ya[b3][:], yb_[b3][:], ALU.add), reads=[("ya", b3), ("yb", b3)], writes=[("ya", b3)])
                p.op("dve", lambda e, b3=b3, b6=b6: e.tensor_tensor(xa[b6][:], xa[b6][:], ya[b3][:], ALU.add), reads=[("xa", b6), ("ya", b3)], writes=[("xa", b6)])
                p.op("act", lambda e, b6=b6, tt=tt: e.activation(junk[:], xa[b6][:], AF.Square, accum_out=st3[:, tt:tt + 1]), reads=[("xa", b6), "st3z"], writes=["junkD", ("ss3", tt)])
            gs = slice(4 * g4, 4 * g4 + 4)
            st3g = st3[:, gs]
            p.op("dve", lambda e, st3g=st3g: e.tensor_scalar(st3g, st3g, 1.0 / D, EPS, ALU.mult, ALU.add), reads=[("ss3", 4 * g4 + i) for i in range(4)], writes=[("st3", g4)])
            p.op("act", lambda e, st3g=st3g: e.activation(st3g, st3g, AF.Ln), reads=[("st3", g4)], writes=[("st3", g4)])
            p.op("act", lambda e, st3g=st3g: e.activation(st3g, st3g, AF.Exp, scale=-0.5), reads=[("st3", g4)], writes=[("st3", g4)])
            for i in range(4):
                tt = 4 * g4 + i
                b3 = tt % 3
                b6 = tt % 6
                p.op("dve", lambda e, b3=b3, b6=b6, tt=tt: e.scalar_tensor_tensor(outt[b3][:], xa[b6][:], st3[:, tt:tt + 1], gfbc[:], ALU.mult, ALU.mult),
                     reads=[("xa", b6), ("st3", g4), "gfbc"], writes=[("outt", b3)])
                finals.append(p.dma("sp", y_out[tt * 128:(tt + 1) * 128, :], outt[b3][:], reads=[("outt", b3)]))

    phase_D()

    p.finals = finals
    p.finalize()
    p.run_block()
    info = dict(n_ops=len(p.ops), max_sem=p.max_sem, sbuf_peak=sb.peak - sb.base)
    return nc, info


_CACHE = {}


def _consts():
    identb = np.eye(128, dtype=np.float32).astype(ml_dtypes.bfloat16)
    identf = np.eye(128, dtype=np.float32)
    s = np.arange(128)[:, None]
    t = np.arange(128)[None, :]
    tri = (s <= t).astype(np.float32)
    mneg = np.where(s <= t, 0.0, NEGM).astype(np.float32).astype(ml_dtypes.bfloat16)
    tris = (s < t).astype(np.float32)
    return identb, identf, tri, mneg, tris


def run_layer(inputs, NPREV, NOWN, debug=False):
    x = np.asarray(inputs["x"], dtype=np.float32)
    B, T, _ = x.shape
    nhalf = T // NOWN
    ncores = B * nhalf
    key = (NPREV, NOWN, debug)
    if key not in _CACHE:
        _CACHE[key] = build_program(NPREV, NOWN, debug=debug)
    nc, info = _CACHE[key]
    f = lambda k: np.ascontiguousarray(np.asarray(inputs[k], dtype=np.float32)[0])
    identb, identf, tri, mneg, tris = _consts()
    btab, bmsk, bneg = _bias_tables(np.asarray(inputs["rel_bias"], dtype=np.float32))
    shared = {
        "w_in": f("w_in"), "b_forget": f("b_forget"), "attn_norm": f("attn_norm"),
        "out_norm_dil": f("out_norm_dil"), "out_norm_fox": f("out_norm_fox"), "w_out": f("w_out"),
        "ffn_norm": f("ffn_norm"), "w_rg": f("w_router_group"), "b_rg": f("b_router_group"),
        "w_re": f("w_router_expert"), "b_re": f("b_router_expert"), "w_eg": f("w_expert_gate"),
        "w_eu": f("w_expert_up"), "w_ed": f("w_expert_down"),
        "final_norm": np.ascontiguousarray(np.asarray(inputs["final_norm"], dtype=np.float32)),
        "btab": btab, "bmsk": bmsk, "bneg": bneg, "identb": identb, "identf": identf, "tri": tri,
        "mneg": mneg, "tris": tris,
    }
    NTO_ = NOWN // 128
    NTL_ = 2 * NOWN // 128 + 32
    shared["tokid"] = (np.arange(NTO_)[None, :] * 128 + np.arange(128)[:, None]).astype(np.float32)
    shared["jv"] = np.broadcast_to((np.arange(NTL_) * 128.0)[None, :], (128, NTL_)).astype(np.float32).copy()
    shared["pidx"] = np.arange(128, dtype=np.float32).reshape(128, 1)
    si = np.zeros((NTL_ * 128, 4), np.int32)
    si[:, 1] = 2 * NOWN + np.arange(NTL_ * 128)
    shared["slot_init"] = si
    in_maps = []
    for core in range(ncores):
        b, hidx = divmod(core, nhalf)
        own0 = hidx * NOWN
        xs = np.zeros((NPREV + NOWN, D), np.float32)
        npv = min(NPREV, own0)
        if npv > 0:
            xs[NPREV - npv:NPREV] = x[b, own0 - npv:own0]
        xs[NPREV:] = x[b, own0:own0 + NOWN]
        pv = 1.0 if npv == NPREV else 0.0
        assert npv in (0, NPREV)
        m = dict(shared)
        m["xs"] = xs
        m["pvt"] = np.full((128, 64), pv, np.float32)
        in_maps.append(m)
    res = run_bass_kernel_spmd(nc, in_maps, core_ids=list(range(ncores)))
    out = np.zeros((B, T, D), np.float32)
    for core in range(ncores):
        b, hidx = divmod(core, nhalf)
        out[b, hidx * NOWN:(hidx + 1) * NOWN] = res.results[core]["y"]
    return out, res, info


def kernel(**inputs):
    out, _, _ = run_layer(inputs, 4096, 4096)
    return out
```
